# Optimizing a Trainium2 kernel written in Bass

```python
import jax
import jax.numpy as jnp
from jax import lax
import numpy as np

D_MODEL = 1024
BATCH = 8
SEQ = 2048
DEPTH = 2

GRID_W = 64
CTX_LEN = 256
N_HEADS = 8
QK_NOPE = 64
QK_ROPE = 32
QK_HEAD = QK_NOPE + QK_ROPE
V_HEAD = 64
Q_LORA = 384
KV_LORA = 256
ROPE_BASE = 10000.0
Q_BLOCK = 128
CONV_CH = 256
CONV_WIDTH = 31
CONV_PAD = (CONV_WIDTH - 1) // 2
FOURIER_GROUPS = 4
FOURIER_GROUP_DIM = 64
FOURIER_CH = FOURIER_GROUPS * FOURIER_GROUP_DIM
ATTN_OUT = N_HEADS * V_HEAD
MIX_WIDTH = ATTN_OUT + CONV_CH + FOURIER_CH
OFF_CQ = 0
OFF_CKV = OFF_CQ + Q_LORA
OFF_KR = OFF_CKV + KV_LORA
OFF_CONV = OFF_KR + QK_ROPE
OFF_FOUR = OFF_CONV + 2 * CONV_CH
IN_COLS = OFF_FOUR + FOURIER_CH
D_FF = 2816
N_EXPERTS = 8
TOP_K = 2
N_DENSE = (DEPTH + 1) // 2
N_MOE = DEPTH // 2
N_MOD = 6
EPS = 1e-6

kernel_name = 'hybrid_mla_conformer_fnet_moe_dit'


def _rmsnorm(x, g):
    xf = x.astype(jnp.float32)
    y = xf * lax.rsqrt(jnp.mean(xf * xf, axis=-1, keepdims=True) + EPS)
    return (y * g.astype(jnp.float32)).astype(x.dtype)


def _layernorm(x, g, b):
    xf = x.astype(jnp.float32)
    mu = jnp.mean(xf, axis=-1, keepdims=True)
    var = jnp.mean(jnp.square(xf - mu), axis=-1, keepdims=True)
    y = (xf - mu) * lax.rsqrt(var + EPS)
    return (y * g.astype(jnp.float32) + b.astype(jnp.float32)).astype(x.dtype)


def _modulation(cvec, w, b):
    m = jax.nn.silu(cvec) @ w + b
    return [p[:, None, :] for p in jnp.split(m, N_MOD, axis=-1)]


def _adaln(x, g, shift, scale):
    return _rmsnorm(x, g) * (1 + scale) + shift


def _axial_rope(rows):
    row = jnp.repeat(jnp.arange(rows, dtype=jnp.float32), GRID_W)
    col = jnp.tile(jnp.arange(GRID_W, dtype=jnp.float32), rows)
    n_freq = QK_ROPE // 4
    inv = ROPE_BASE ** (-jnp.arange(n_freq, dtype=jnp.float32) / n_freq)
    ang = jnp.concatenate([row[:, None] * inv, col[:, None] * inv], axis=-1)
    return jnp.cos(ang), jnp.sin(ang)


def _apply_rope(x, rope):
    if rope is None:
        return x
    cos, sin = rope
    cos = cos[:, None, :]
    sin = sin[:, None, :]
    xf = x.astype(jnp.float32)
    x1, x2 = jnp.split(xf, 2, axis=-1)
    return jnp.concatenate([x1 * cos - x2 * sin, x2 * cos + x1 * sin], axis=-1).astype(x.dtype)


def _mla_queries(cq, w_uq, q_lat_g, q_norm_g, rope):
    b, s, _ = cq.shape
    q = (_rmsnorm(cq, q_lat_g) @ w_uq).reshape(b, s, N_HEADS, QK_HEAD)
    q = _rmsnorm(q, q_norm_g)
    return jnp.concatenate([q[..., :QK_NOPE], _apply_rope(q[..., QK_NOPE:], rope)], axis=-1)


def _mla_keys_values(ckv, kr, w_ukv, kv_lat_g, k_norm_g, rope):
    b, s, _ = ckv.shape
    kv = (_rmsnorm(ckv, kv_lat_g) @ w_ukv).reshape(b, s, N_HEADS, QK_NOPE + V_HEAD)
    k_rope = jnp.broadcast_to(kr[:, :, None, :], (b, s, N_HEADS, QK_ROPE))
    k = _rmsnorm(jnp.concatenate([kv[..., :QK_NOPE], k_rope], axis=-1), k_norm_g)
    k = jnp.concatenate([k[..., :QK_NOPE], _apply_rope(k[..., QK_NOPE:], rope)], axis=-1)
    return k, kv[..., QK_NOPE:]


def _attend(q, k, v):
    b, s, h, d = q.shape
    nb = s // Q_BLOCK
    scale = d ** -0.5
    qb = jnp.moveaxis(q.reshape(b, nb, Q_BLOCK, h, d), 1, 0)

    def block(qblk):
        sc = jnp.einsum('bqhd,bkhd->bhqk', qblk, k).astype(jnp.float32) * scale
        p = jax.nn.softmax(sc, axis=-1).astype(v.dtype)
        return jnp.einsum('bhqk,bkhd->bqhd', p, v)

    o = lax.map(block, qb)
    return jnp.moveaxis(o, 0, 1).reshape(b, s, h * v.shape[-1])


def _conformer_conv(u, conv_w, conv_b, ln_g, ln_b):
    a, g = jnp.split(u, 2, axis=-1)
    y = a * jax.nn.sigmoid(g)
    y = lax.conv_general_dilated(
        y, conv_w[:, None, :], window_strides=(1,), padding=[(CONV_PAD, CONV_PAD)],
        dimension_numbers=('NWC', 'WIO', 'NWC'), feature_group_count=CONV_CH) + conv_b
    return jax.nn.silu(_layernorm(y, ln_g, ln_b))


def _fourier_mix(u):
    b, s, _ = u.shape
    z = u.astype(jnp.float32).reshape(b, s, FOURIER_GROUPS, FOURIER_GROUP_DIM)
    f = jnp.real(jnp.fft.fft2(z, axes=(1, 3), norm='ortho'))
    return f.reshape(b, s, FOURIER_CH).astype(u.dtype)


def _mixer_out(cols, k, v, rope, w_uq, q_lat_g, q_norm_g, conv_w, conv_b, conv_ln_g, conv_ln_b, w_out):
    q = _mla_queries(cols[..., OFF_CQ:OFF_CKV], w_uq, q_lat_g, q_norm_g, rope)
    attn = _attend(q, k, v)
    conv = _conformer_conv(cols[..., OFF_CONV:OFF_FOUR], conv_w, conv_b, conv_ln_g, conv_ln_b)
    four = _fourier_mix(cols[..., OFF_FOUR:IN_COLS])
    return jnp.concatenate([attn, conv, four], axis=-1) @ w_out


def _swiglu(h, wg, wu, wd):
    return (jax.nn.silu(h @ wg) * (h @ wu)) @ wd


def _moe_swiglu(h, router_w, wg, wu, wd):
    b, s, d = h.shape
    t = h.reshape(b * s, d)
    logits = (t @ router_w).astype(jnp.float32)
    top_v, top_i = lax.top_k(logits, TOP_K)
    w = jax.nn.softmax(top_v, axis=-1)
    gates = jnp.einsum('nk,nke->ne', w, jax.nn.one_hot(top_i, N_EXPERTS, dtype=jnp.float32)).astype(h.dtype)
    out = jnp.zeros_like(t)
    for e in range(N_EXPERTS):
        out = out + gates[:, e:e + 1] * _swiglu(t, wg[e], wu[e], wd[e])
    return out.reshape(b, s, d)


def _channel_mixer(h, layer, ffn_w_gate, ffn_w_up, ffn_w_down, router_w, moe_w_gate, moe_w_up, moe_w_down):
    i = layer // 2
    if layer % 2 == 0:
        return _swiglu(h, ffn_w_gate[i], ffn_w_up[i], ffn_w_down[i])
    return _moe_swiglu(h, router_w[i], moe_w_gate[i], moe_w_up[i], moe_w_down[i])


def _normal(k, shape, scale):
    return jax.random.normal(k, shape, jnp.float32) * scale


def setup_inputs(seed: int = 0) -> dict:
    key = jax.random.key(seed)
    ks = jax.random.split(key, 32)
    D = D_MODEL
    return {
        'x': _normal(ks[0], (BATCH, SEQ, D), 1.0),
        'c': _normal(ks[1], (BATCH, D), 1.0),
        'ctx': _normal(ks[2], (BATCH, CTX_LEN, D), 1.0),
        'c_ctx': _normal(ks[3], (D,), 1.0),
        'ada_w': _normal(ks[4], (DEPTH, D, N_MOD * D), 0.5 * D ** -0.5),
        'ada_b': _normal(ks[5], (DEPTH, N_MOD * D), 0.02),
        'mix_norm_g': 1.0 + _normal(ks[6], (DEPTH, D), 0.05),
        'ffn_norm_g': 1.0 + _normal(ks[7], (DEPTH, D), 0.05),
        'w_in': _normal(ks[8], (DEPTH, D, IN_COLS), D ** -0.5),
        'q_lat_g': 1.0 + _normal(ks[9], (DEPTH, Q_LORA), 0.05),
        'kv_lat_g': 1.0 + _normal(ks[10], (DEPTH, KV_LORA), 0.05),
        'w_uq': _normal(ks[11], (DEPTH, Q_LORA, N_HEADS * QK_HEAD), Q_LORA ** -0.5),
        'w_ukv': _normal(ks[12], (DEPTH, KV_LORA, N_HEADS * (QK_NOPE + V_HEAD)), KV_LORA ** -0.5),
        'q_norm_g': 1.0 + _normal(ks[13], (DEPTH, QK_HEAD), 0.05),
        'k_norm_g': 1.0 + _normal(ks[14], (DEPTH, QK_HEAD), 0.05),
        'conv_w': _normal(ks[15], (DEPTH, CONV_WIDTH, CONV_CH), CONV_WIDTH ** -0.5),
        'conv_b': _normal(ks[16], (DEPTH, CONV_CH), 0.02),
        'conv_ln_g': 1.0 + _normal(ks[17], (DEPTH, CONV_CH), 0.05),
        'conv_ln_b': _normal(ks[18], (DEPTH, CONV_CH), 0.02),
        'w_out': _normal(ks[19], (DEPTH, MIX_WIDTH, D), MIX_WIDTH ** -0.5),
        'ffn_w_gate': _normal(ks[20], (N_DENSE, D, D_FF), D ** -0.5),
        'ffn_w_up': _normal(ks[21], (N_DENSE, D, D_FF), D ** -0.5),
        'ffn_w_down': _normal(ks[22], (N_DENSE, D_FF, D), D_FF ** -0.5),
        'router_w': _normal(ks[23], (N_MOE, D, N_EXPERTS), D ** -0.5),
        'moe_w_gate': _normal(ks[24], (N_MOE, N_EXPERTS, D, D_FF), D ** -0.5),
        'moe_w_up': _normal(ks[25], (N_MOE, N_EXPERTS, D, D_FF), D ** -0.5),
        'moe_w_down': _normal(ks[26], (N_MOE, N_EXPERTS, D_FF, D), D_FF ** -0.5),
    }


def reference(x, c, ctx, c_ctx, ada_w, ada_b, mix_norm_g, ffn_norm_g, w_in, q_lat_g, kv_lat_g, w_uq, w_ukv,
              q_norm_g, k_norm_g, conv_w, conv_b, conv_ln_g, conv_ln_b, w_out, ffn_w_gate, ffn_w_up, ffn_w_down,
              router_w, moe_w_gate, moe_w_up, moe_w_down):
    rows = x.shape[1] // GRID_W
    rope = _axial_rope(rows)
    for l in range(DEPTH):
        last = l == DEPTH - 1
        sx = _modulation(c, ada_w[l], ada_b[l])
        sc = _modulation(c_ctx[None, :], ada_w[l], ada_b[l])

        hc = _adaln(ctx, mix_norm_g[l], sc[0], sc[1])
        if last:
            kv_cols = hc @ w_in[l][:, OFF_CKV:OFF_CONV]
            k_c, v_c = _mla_keys_values(kv_cols[..., :KV_LORA], kv_cols[..., KV_LORA:],
                                        w_ukv[l], kv_lat_g[l], k_norm_g[l], None)
        else:
            cols_c = hc @ w_in[l]
            k_c, v_c = _mla_keys_values(cols_c[..., OFF_CKV:OFF_KR], cols_c[..., OFF_KR:OFF_CONV],
                                        w_ukv[l], kv_lat_g[l], k_norm_g[l], None)
            mix_c = _mixer_out(cols_c, k_c, v_c, None, w_uq[l], q_lat_g[l], q_norm_g[l],
                               conv_w[l], conv_b[l], conv_ln_g[l], conv_ln_b[l], w_out[l])
            ctx_next = ctx + sc[2] * mix_c
            ctx_next = ctx_next + sc[5] * _channel_mixer(
                _adaln(ctx_next, ffn_norm_g[l], sc[3], sc[4]), l,
                ffn_w_gate, ffn_w_up, ffn_w_down, router_w, moe_w_gate, moe_w_up, moe_w_down)

        hx = _adaln(x, mix_norm_g[l], sx[0], sx[1])
        cols_x = hx @ w_in[l]
        k_x, v_x = _mla_keys_values(cols_x[..., OFF_CKV:OFF_KR], cols_x[..., OFF_KR:OFF_CONV],
                                    w_ukv[l], kv_lat_g[l], k_norm_g[l], rope)
        k_all = jnp.concatenate([k_x, k_c], axis=1)
        v_all = jnp.concatenate([v_x, v_c], axis=1)
        mix_x = _mixer_out(cols_x, k_all, v_all, rope, w_uq[l], q_lat_g[l], q_norm_g[l],
                           conv_w[l], conv_b[l], conv_ln_g[l], conv_ln_b[l], w_out[l])
        x = x + sx[2] * mix_x
        x = x + sx[5] * _channel_mixer(
            _adaln(x, ffn_norm_g[l], sx[3], sx[4]), l,
            ffn_w_gate, ffn_w_up, ffn_w_down, router_w, moe_w_gate, moe_w_up, moe_w_down)

        if not last:
            ctx = ctx_next
    return x
```

```python
import numpy as np
import ml_dtypes
from bisect import bisect_left
import concourse.bass as bass
import concourse.mybir as mybir
from concourse.bass_utils import run_bass_kernel_spmd

F32 = mybir.dt.float32
BF16 = mybir.dt.bfloat16
AF = mybir.ActivationFunctionType
ALU = mybir.AluOpType
ES = {F32: 4, BF16: 2}

D = 1024
SEQ = 2048
CTX = 256
TT = SEQ + CTX
NH = 8
DFF = 2816
NE = 8
EPS = 1e-6
SB_LO = 16512
SB_HI = 229344


class V:
    __slots__ = ("ap", "sp", "lo", "hi")

    def __init__(s, ap, sp, lo, hi):
        s.ap, s.sp, s.lo, s.hi = ap, sp, lo, hi

    def w(s, fn):
        return V(fn(s.ap), s.sp, s.lo, s.hi)


class Tn:
    def __init__(s, t, shape, dtype, space, base):
        s.t, s.shape, s.dtype, s.space, s.base = t, list(shape), dtype, space, base
        s.es = ES[dtype]
        st = [1] * len(shape)
        for i in range(len(shape) - 2, 0, -1):
            st[i] = st[i + 1] * shape[i + 1]
        s.st = st
        s.nbytes = int(np.prod(shape[1:])) * s.es

    def __getitem__(s, idx):
        if not isinstance(idx, tuple):
            idx = (idx,)
        ap = s.t[idx]
        lo = 0
        hi = 0
        for d in range(1, len(s.shape)):
            n = s.shape[d]
            if d < len(idx):
                ix = idx[d]
                if isinstance(ix, slice):
                    a, b, c = ix.indices(n)
                    last = a + ((b - a - 1) // c) * c
                else:
                    a = ix
                    last = ix
            else:
                a, last = 0, n - 1
            lo += a * s.st[d]
            hi += last * s.st[d]
        lo_b = s.base + lo * s.es
        hi_b = s.base + (hi + 1) * s.es
        if s.space == "PS":
            lo_b = (lo_b // 2048) * 2048
            hi_b = ((hi_b + 2047) // 2048) * 2048
        return V(ap, s.space, lo_b, hi_b)


class Segs:
    def __init__(s):
        s.b = [0, 1 << 40]
        s.w = [None]
        s.r = [{}]

    def _split(s, x):
        i = bisect_left(s.b, x)
        if s.b[i] == x:
            return
        s.b.insert(i, x)
        s.w.insert(i, s.w[i - 1])
        s.r.insert(i, dict(s.r[i - 1]))

    def access(s, lo, hi, idx, key, write, deps):
        s._split(lo)
        s._split(hi)
        i = bisect_left(s.b, lo)
        while s.b[i] < hi:
            if s.w[i] is not None:
                deps.add(s.w[i])
            if write:
                for v in s.r[i].values():
                    deps.add(v)
                s.w[i] = idx
                s.r[i] = {}
            else:
                s.r[i][key] = idx
            i += 1


class Chan:
    def __init__(s, sem, bulk, name):
        s.sem, s.bulk, s.count, s.name = sem, bulk, 0, name


class Ins:
    __slots__ = ("eng", "fn", "deps", "chan", "cval", "signal", "sigval")


class Prog:
    def __init__(s, nc):
        s.nc = nc
        s.E = {"pe": nc.tensor, "act": nc.scalar, "dve": nc.vector, "pool": nc.gpsimd, "sp": nc.sync}
        s.ins = []
        s.segs = {"SB": Segs(), "PS": Segs()}
        s.sems = {e: nc.alloc_semaphore("s_" + e) for e in ("pe", "act", "dve", "pool")}
        s.chans = {}
        s.free = [(SB_LO, SB_HI)]
        s.nalloc = 0
        s.ps_t = nc.alloc_psum_tensor("psall", [128, 8, 512], F32)
        s.ps = Tn(s.ps_t, [128, 8, 512], F32, "PS", 0)

    def alloc(s, name, shape, dtype):
        nb = int(np.prod(shape[1:])) * ES[dtype]
        nb = (nb + 63) // 64 * 64
        for i, (a, b) in enumerate(s.free):
            if b - a >= nb:
                s.free[i] = (a + nb, b)
                if s.free[i][0] == s.free[i][1]:
                    s.free.pop(i)
                s.nalloc += 1
                t = s.nc.alloc_sbuf_tensor_at("%s_%d" % (name, s.nalloc), list(shape), dtype, offset=a)
                tn = Tn(t, shape, dtype, "SB", a)
                tn.size = nb
                return tn
        raise RuntimeError("SBUF alloc failed for %s (%d B); free=%s" % (name, nb, s.free))

    def release(s, *tns):
        for tn in tns:
            s.free.append((tn.base, tn.base + tn.size))
        s.free.sort()
        m = []
        for a, b in s.free:
            if m and m[-1][1] == a:
                m[-1] = (m[-1][0], b)
            else:
                m.append((a, b))
        s.free = m

    def chan(s, name, bulk=False):
        if name not in s.chans:
            s.chans[name] = Chan(s.nc.alloc_semaphore("c_" + name), bulk, name)
        return s.chans[name]

    def op(s, eng, fn, reads=(), writes=(), chan=None):
        ins = Ins()
        ins.eng, ins.fn, ins.chan, ins.signal, ins.sigval, ins.cval = eng, fn, chan, False, 0, 0
        idx = len(s.ins)
        deps = set()
        key = eng if chan is None else "c:" + chan.name
        for v in reads:
            if isinstance(v, V):
                s.segs[v.sp].access(v.lo, v.hi, idx, key, v.sp == "PS", deps)
        for v in writes:
            if isinstance(v, V):
                s.segs[v.sp].access(v.lo, v.hi, idx, key, True, deps)
        if chan is not None:
            chan.count += 16
            ins.cval = chan.count
        deps.discard(idx)
        ins.deps = deps
        s.ins.append(ins)
        return idx

    def emit(s):
        ins = s.ins
        need = []
        for i, x in enumerate(ins):
            nd = {}
            for j in x.deps:
                y = ins[j]
                if y.chan is not None:
                    k = ("c", y.chan.name)
                    nd[k] = max(nd.get(k, -1), j)
                else:
                    if y.eng == "pe" and x.eng == "pe" and x.chan is None:
                        continue
                    k = ("e", y.eng)
                    nd[k] = max(nd.get(k, -1), j)
            need.append(nd)
            for k, j in nd.items():
                if k[0] == "e":
                    ins[j].signal = True
        cnt = {e: 0 for e in s.sems}
        for x in ins:
            if x.signal:
                cnt[x.eng] += 1
                x.sigval = cnt[x.eng]
        waited = {e: {} for e in s.E}
        nw = 0
        for i, x in enumerate(ins):
            eng = s.E[x.eng]
            wd = waited[x.eng]
            for k, j in need[i].items():
                y = ins[j]
                if k[0] == "c":
                    sem = y.chan.sem
                    val = y.chan.count if y.chan.bulk else y.cval
                else:
                    sem = s.sems[y.eng]
                    val = y.sigval
                sid = id(sem)
                if wd.get(sid, 0) >= val:
                    continue
                wd[sid] = val
                eng.wait_ge(sem, val)
                nw += 1
            bi = x.fn()
            if x.chan is not None:
                bi.then_inc(x.chan.sem, 16)
            elif x.signal:
                bi.then_inc(s.sems[x.eng], 1)
        return nw

    def mm(s, out, lhsT, rhs, start, stop):
        nc = s.nc
        return s.op("pe", lambda: nc.tensor.matmul(out.ap, lhsT=lhsT.ap, rhs=rhs.ap, start=start, stop=stop),
                    reads=[lhsT, rhs], writes=[out])

    def transpose(s, out, in_, ident):
        nc = s.nc
        return s.op("pe", lambda: nc.tensor.transpose(out.ap, in_.ap, ident.ap), reads=[in_, ident], writes=[out])

    def act(s, out, in_, func, scale=None, bias=None):
        nc = s.nc
        kw = {}
        rd = [in_]
        if scale is not None:
            if isinstance(scale, V):
                kw["scale"] = scale.ap
                rd.append(scale)
            else:
                kw["scale"] = float(scale)
        if bias is not None:
            if isinstance(bias, V):
                kw["bias"] = bias.ap
                rd.append(bias)
            else:
                kw["bias"] = float(bias)
        return s.op("act", lambda: nc.scalar.activation(out=out.ap, in_=in_.ap, func=func, **kw), reads=rd, writes=[out])

    def tt(s, out, a, b, op, eng="dve"):
        e = s.E[eng]
        return s.op(eng, lambda: e.tensor_tensor(out=out.ap, in0=a.ap, in1=b.ap, op=op), reads=[a, b], writes=[out])

    def ts(s, out, a, s1, op0, s2=None, op1=None, eng="dve"):
        e = s.E[eng]
        rd = [a]
        a1 = s1.ap if isinstance(s1, V) else float(s1)
        if isinstance(s1, V):
            rd.append(s1)
        if op1 is None:
            return s.op(eng, lambda: e.tensor_scalar(out=out.ap, in0=a.ap, scalar1=a1, scalar2=None, op0=op0),
                        reads=rd, writes=[out])
        a2 = s2.ap if isinstance(s2, V) else float(s2)
        if isinstance(s2, V):
            rd.append(s2)
        return s.op(eng, lambda: e.tensor_scalar(out=out.ap, in0=a.ap, scalar1=a1, scalar2=a2, op0=op0, op1=op1),
                    reads=rd, writes=[out])

    def stt(s, out, a, sc, b, op0, op1):
        nc = s.nc
        rd = [a, b]
        a1 = sc.ap if isinstance(sc, V) else float(sc)
        if isinstance(sc, V):
            rd.append(sc)
        return s.op("dve", lambda: nc.vector.scalar_tensor_tensor(out=out.ap, in0=a.ap, scalar=a1, in1=b.ap, op0=op0, op1=op1),
                    reads=rd, writes=[out])

    def copy(s, out, in_, eng="dve"):
        if eng == "act":
            return s.act(out, in_, AF.Copy)
        e = s.E[eng]
        return s.op(eng, lambda: e.tensor_copy(out=out.ap, in_=in_.ap), reads=[in_], writes=[out])

    def recip(s, out, in_):
        nc = s.nc
        return s.op("dve", lambda: nc.vector.reciprocal(out=out.ap, in_=in_.ap), reads=[in_], writes=[out])

    def memset(s, out, val, eng="dve"):
        e = s.E[eng]
        return s.op(eng, lambda: e.memset(out.ap, val), writes=[out])

    def dma(s, q, out, in_, chan):
        e = s.E[q]
        oa = out.ap if isinstance(out, V) else out
        ia = in_.ap if isinstance(in_, V) else in_
        return s.op(q, lambda: e.dma_start(out=oa, in_=ia), reads=[in_], writes=[out], chan=chan)


def _col(v, n):
    v = np.asarray(v, np.float32).reshape(-1)
    pad = n * 128 - v.size
    if pad:
        v = np.concatenate([v, np.zeros(pad, np.float32)])
    return v.reshape(n, 128).T


VL = 139
V_MIXG, V_FFNG, V_ADAB, V_QLAT, V_KVLAT, V_QN, V_KN, V_CB, V_LNG, V_LNB, V_CW = 0, 8, 16, 64, 67, 69, 70, 71, 73, 75, 77


def pack_vecs(inp, b):
    cols = [_col(inp["c"][b], 8), _col(inp["c_ctx"], 8)]
    for l in range(2):
        cols += [_col(inp["mix_norm_g"][l], 8), _col(inp["ffn_norm_g"][l], 8), _col(inp["ada_b"][l], 48),
                 _col(inp["q_lat_g"][l], 3), _col(inp["kv_lat_g"][l], 2), _col(inp["q_norm_g"][l], 1),
                 _col(inp["k_norm_g"][l], 1), _col(inp["conv_b"][l], 2), _col(inp["conv_ln_g"][l], 2),
                 _col(inp["conv_ln_b"][l], 2)]
        cw = np.asarray(inp["conv_w"][l], np.float32)
        cols += [cw[:, 0:128].T, cw[:, 128:256].T]
    return np.ascontiguousarray(np.concatenate(cols, axis=1), dtype=np.float32)


_CONST = {}


def consts():
    if _CONST:
        return _CONST
    bf = ml_dtypes.bfloat16
    ident = np.eye(128, dtype=np.float32)
    perm = np.zeros((128, 96), np.float32)
    for i in range(16):
        perm[80 + i, 64 + i] = -1.0
        perm[64 + i, 80 + i] = 1.0
    k = np.arange(64)
    ang = 2 * np.pi * np.outer(k, k) / 64.0
    cc = np.zeros((128, 128)); scn = np.zeros((128, 128))
    for g in range(2):
        cc[g * 64:(g + 1) * 64, g * 64:(g + 1) * 64] = np.cos(ang)
        scn[g * 64:(g + 1) * 64, g * 64:(g + 1) * 64] = -np.sin(ang)
    cb = np.concatenate([ident, np.ones((128, 128)), perm, cc, scn], axis=1).astype(bf)
    t = np.arange(SEQ)
    row = (t // 64).astype(np.float64); col = (t % 64).astype(np.float64)
    inv = 10000.0 ** (-np.arange(8, dtype=np.float64) / 8)
    a = np.concatenate([row[:, None] * inv, col[:, None] * inv], axis=1)
    C = np.ones((96, TT)); S = np.zeros((96, TT))
    C[64:80, :SEQ] = np.cos(a).T; C[80:96, :SEQ] = np.cos(a).T
    S[64:80, :SEQ] = np.sin(a).T; S[80:96, :SEQ] = np.sin(a).T
    rope = np.concatenate([C, S], axis=1).astype(bf)
    s = np.arange(SEQ, dtype=np.float64)
    m = np.outer(s, s) % SEQ
    dc = np.cos(2 * np.pi * m / SEQ).astype(bf)
    ds = np.sin(2 * np.pi * m / SEQ).astype(bf)
    s2 = np.arange(CTX, dtype=np.float64)
    m2 = np.outer(s2, s2) % CTX
    d256 = np.concatenate([np.cos(2 * np.pi * m2 / CTX), np.sin(2 * np.pi * m2 / CTX)], axis=1).astype(bf)
    sel = np.zeros((8, 8, 128), np.float32)
    for e in range(8):
        sel[e, e, :] = 1.0
    _CONST.update(dict(identf=ident, cb=cb, rope=rope, dftc=dc, dfts=ds, dft256=d256, sel=sel.reshape(8, 1024)))
    return _CONST


XBLK = [(0, 512), (512, 512), (1024, 512), (1536, 512)]
CBLK = (2048, 256)


def build(dump=None, nlayers=2, moe_dense=True):
    nc = bass.Bass("TRN2", target_bir_lowering=False)
    P = Prog(nc)

    def din(name, shape, dt=F32):
        return nc.dram_tensor(name, list(shape), dt, kind="ExternalInput").ap()

    x_d = din("x", [SEQ, D]); ctx_d = din("ctx", [CTX, D]); vecs_d = din("vecs", [128, 16 + 2 * VL])
    identf_d = din("identf", [128, 128]); cb_d = din("cb", [128, 608], BF16); rope_d = din("rope", [96, 2 * TT], BF16)
    dftc_d = din("dftc", [SEQ, SEQ], BF16); dfts_d = din("dfts", [SEQ, SEQ], BF16); d256_d = din("dft256", [CTX, 512], BF16)
    sel_d = din("sel", [8, 1024])
    ada_w_d = din("ada_w", [2, D, 6 * D]); w_in_d = din("w_in", [2, D, 1440]); w_uq_d = din("w_uq", [2, 384, 768])
    w_ukv_d = din("w_ukv", [2, 256, 1024]); w_out_d = din("w_out", [2, D, D])
    fg_d = din("ffn_w_gate", [1, D, DFF]); fu_d = din("ffn_w_up", [1, D, DFF]); fd_d = din("ffn_w_down", [1, DFF, D])
    rw_d = din("router_w", [1, D, NE])
    mg_d = din("moe_w_gate", [1, NE, D, DFF]); mu_d = din("moe_w_up", [1, NE, D, DFF]); md_d = din("moe_w_down", [1, NE, DFF, D])
    out_d = nc.dram_tensor("out", [SEQ, D], F32, kind="ExternalOutput").ap()
    dumps = {}

    ps = P.ps
    cconst = P.chan("const", bulk=True)

    xT = P.alloc("xT", [128, 8, SEQ], F32)
    cT = P.alloc("cT", [128, 8, CTX], F32)
    vecs = P.alloc("vecs", [128, 16 + 2 * VL], F32)
    identf = P.alloc("identf", [128, 128], F32)
    cb = P.alloc("cb", [128, 608], BF16)
    rope = P.alloc("rope", [96, 2 * TT], BF16)
    d256 = P.alloc("d256", [128, 2, 512], BF16)
    sc2 = P.alloc("sc2", [128, 8, 2], BF16)
    mod = P.alloc("mod", [128, 48, 2], F32)
    gsm = P.alloc("gsm", [128, 8, 2], F32)
    gsf = P.alloc("gsf", [128, 8, 2], F32)
    P.dma("sp", vecs[:, :], vecs_d, cconst)
    P.dma("sp", identf[:, :], identf_d, cconst)
    P.dma("sp", cb[:, :], cb_d, cconst)
    P.dma("sp", rope[:, :], rope_d, cconst)
    P.dma("sp", d256[:, :, :], d256_d.rearrange("(i p) c -> p i c", p=128), cconst)
    identb = lambda a=128, b=128: cb[0:a, 0:b]
    ones = lambda a=128, b=128: cb[0:a, 128:128 + b]
    permv = cb[0:96, 256:352]
    ccsc = cb[:, 352:608]

    def vcol(l, off, n=1, rows=128):
        c0 = 16 + l * VL + off
        return vecs[0:rows, c0:c0 + n]

    xin = [P.alloc("xin", [128, D], F32) for _ in range(2)]
    cx = [P.chan("xin0"), P.chan("xin1")]
    tiles = [(x_d, i, xT, i * 128) for i in range(16)] + [(ctx_d, i, cT, i * 128) for i in range(2)]
    for n, (src, i, dst, t0) in enumerate(tiles):
        xb = xin[n % 2]
        P.dma("sp", xb[:, :], src[i * 128:(i + 1) * 128, :], cx[n % 2])
        for half in range(2):
            bk = (2 * n + half) % 8
            for q in range(4):
                dc = half * 4 + q
                P.transpose(ps[:, bk, q * 128:(q + 1) * 128], xb[:, dc * 128:(dc + 1) * 128], identf[:, :])
            src_v = ps[:, bk, :].w(lambda a: a.rearrange("p (q t) -> p q t", q=4))
            P.copy(dst[:, half * 4:half * 4 + 4, t0:t0 + 128], src_v, eng=("act" if half else "dve"))
    P.release(*xin)
    P.act(sc2[:, :, 0], vecs[:, 0:8], AF.Silu)
    P.act(sc2[:, :, 1], vecs[:, 8:16], AF.Silu)


    def wload(slot_views, srcs, chname, q="pool"):
        ch = P.chan(chname)
        ids = [P.dma(q, v, sap, ch) for v, sap in zip(slot_views, srcs)]
        for i_ in ids:
            P.ins[i_].cval = ch.count
            P.ins[i_].deps -= set(ids)

    def run_stream(jobs):
        if jobs:
            jobs[0][0]()
        for n_ in range(len(jobs)):
            if n_ + 1 < len(jobs):
                jobs[n_ + 1][0]()
            jobs[n_][1]()

    def adaln_block(src, t0s, n, gs, shoff, j, hout, h32=None, ho=0):
        sq = P.alloc("sq", [128, 3, 512], BF16)
        rt = P.alloc("rt", [128, 512], F32)
        rstd = P.alloc("rstd", [128, 512], F32)
        tmp = [P.alloc("tmp", [128, 512], F32) for _ in range(2)]
        bk = 7
        for dc in range(8):
            P.act(sq[:, dc % 3, 0:n], src[:, dc, t0s:t0s + n], AF.Square)
            P.mm(ps[:, bk, 0:n], ones(), sq[:, dc % 3, 0:n], dc == 0, dc == 7)
        rsqrt_to(rstd[:, 0:n], ps[:, bk, 0:n], 128, n, 1.0 / D, rt[:, 0:n])
        for dc in range(8):
            t = tmp[dc % 2] if h32 is None else None
            tv = t[:, 0:n] if h32 is None else h32[:, dc, 0:n]
            P.stt(tv, src[:, dc, t0s:t0s + n], gs[:, dc, j:j + 1], rstd[:, 0:n], ALU.mult, ALU.mult)
            if h32 is None:
                P.act(hout[:, dc, ho:ho + n], tv, AF.Identity, bias=mod[:, shoff + dc, j:j + 1])
            else:
                P.ts(tv, tv, mod[:, shoff + dc, j:j + 1], ALU.add)
                P.copy(hout[:, dc, ho:ho + n], tv, eng="act")
        P.release(sq, rt, rstd, *tmp)

    epsc = P.alloc("epsc", [128, 1], F32)
    P.memset(epsc[:, :], EPS)

    def rsqrt_to(out, ssum, npart, n, inv_n, tmp):
        P.act(tmp, ssum, AF.Ln, scale=inv_n, bias=epsc[0:npart, 0:1])
        P.act(out, tmp, AF.Exp, scale=-0.5)

    def rstd_from(out, ssum, npart, n, inv_n):
        rt = P.alloc("rt2", [128, 512], F32)
        rsqrt_to(out, ssum, npart, n, inv_n, rt[0:npart, 0:n])
        P.release(rt)

    def dump_sb(name, tn, shape):
        if dump is None or name not in dump:
            return
        dd = nc.dram_tensor("dbg_" + name, list(shape), tn.dtype, kind="ExternalOutput").ap()
        dumps[name] = dd
        idx = tuple(slice(0, n) for n in shape)
        P.dma("sp", dd, tn[idx], P.chan("out_" + name))

    dump_sb("xT0", xT, [128, 8, SEQ])

    for l in range(nlayers):
        last = (l == 1)
        streams = [(xT, 0, b0, n, 0) for (b0, n) in XBLK] + [(cT, 2048, 0, 256, 1)]
        adab = [P.alloc("adab", [128, 8, 768], BF16) for _ in range(2)]

        def mk_ada(p):
            sl = adab[p % 2]

            def ld():
                wload([sl[:, :, :]], [ada_w_d[l, :, p * 768:(p + 1) * 768].rearrange("(k p) c -> p k c", p=128)], "adab%d" % (p % 2))

            def cp():
                for cc in range(6):
                    g = p * 6 + cc
                    for k in range(8):
                        P.mm(ps[:, 0, 2 * g:2 * g + 2], sl[:, k, cc * 128:(cc + 1) * 128], sc2[:, k, :], k == 0, k == 7)
            return (ld, cp)

        run_stream([mk_ada(p) for p in range(8)])
        for j in range(2):
            P.tt(mod[:, :, j], ps[:, 0, j:96:2], vcol(l, V_ADAB, 48), ALU.add)
        for j in range(2):
            P.stt(gsm[:, :, j], mod[:, 8:16, j], 1.0, vcol(l, V_MIXG, 8), ALU.add, ALU.mult)
            P.stt(gsf[:, :, j], mod[:, 32:40, j], 1.0, vcol(l, V_FFNG, 8), ALU.add, ALU.mult)
        P.release(*adab)
        dump_sb("mod%d" % l, mod, [128, 48, 2])

        cqn = P.alloc("cqn", [128, 3, TT], BF16)
        ckvn = P.alloc("ckvn", [128, 2, TT], BF16)
        krope = P.alloc("krope", [128, TT], F32)
        uT = P.alloc("uT", [128, 2, TT], BF16)
        ypx = P.alloc("ypx", [128, 2, SEQ + 30], BF16)
        ypc = P.alloc("ypc", [128, 2, CTX + 30], BF16)
        w_in = P.alloc("w_in", [128, 8, 1536], BF16)
        P.memset(w_in[:, :, 1440:1504], 0.0, eng="pool")
        with nc.allow_non_contiguous_dma(reason="kr cols"):
            wload([w_in[:, :, 0:1440], w_in[:, :, 1504:1536]],
                  [w_in_d[l].rearrange("(k p) c -> p k c", p=128),
                   w_in_d[l, :, 640:672].rearrange("(k p) c -> p k c", p=128)], "w_in")
        for yp, T in ((ypx, SEQ), (ypc, CTX)):
            P.memset(yp[:, :, 0:15], 0.0, eng="pool")
            P.memset(yp[:, :, T + 15:T + 30], 0.0, eng="pool")
        hbs = [P.alloc("hb", [128, 8, 512], BF16) for _ in range(2)]
        raw = P.alloc("raw", [128, 5, 512], F32)
        sqr = P.alloc("sqr", [128, 5, 512], BF16)
        rs2 = P.alloc("rs2", [128, 1, 512], F32)
        sg1 = P.alloc("sg", [128, 512], F32)
        sg = [sg1, sg1]
        bkc = [0]

        def nb():
            bkc[0] = (bkc[0] + 1) % 6
            return 1 + bkc[0]

        LAT = ((3, 0, 0, V_QLAT, cqn, 1.0 / 384), (2, 384, 3, V_KVLAT, ckvn, 1.0 / 256))
        for si, (src, tb0, t0s, n, j) in enumerate(streams):
            hb = hbs[si % 2]
            adaln_block(src, t0s, n, gsm, 0, j, hb)
            g0 = tb0 + t0s
            skipq = last and j == 1

            def proj(c0, M):
                bk = nb()
                for dc in range(8):
                    P.mm(ps[0:M, bk, 0:n], w_in[:, dc, c0:c0 + M], hb[:, dc, 0:n], dc == 0, dc == 7)
                return bk

            for (nchk, c00, r0, gcol, dst, invn) in LAT:
                if skipq and nchk == 3:
                    continue
                for c in range(nchk):
                    bk = proj(c00 + c * 128, 128)
                    P.act(sqr[:, r0 + c, 0:n], ps[:, bk, 0:n], AF.Square)
                    P.copy(raw[:, r0 + c, 0:n], ps[:, bk, 0:n], eng="dve")
            bk = proj(1440, 96)
            P.copy(krope[0:96, g0:g0 + n], ps[0:96, bk, 0:n], eng="act")
            if not skipq:
                yp = ypx if j == 0 else ypc
                for c in range(2):
                    bg = proj(928 + c * 128, 128)
                    ba = proj(672 + c * 128, 128)
                    P.act(sg[c][:, 0:n], ps[:, bg, 0:n], AF.Sigmoid)
                    P.tt(yp[:, c, 15 + t0s:15 + t0s + n], ps[:, ba, 0:n], sg[c][:, 0:n], ALU.mult)
                for c in range(2):
                    bk = proj(1184 + c * 128, 128)
                    P.copy(uT[:, c, g0:g0 + n], ps[:, bk, 0:n], eng="act")
            for (nchk, c00, r0, gcol, dst, invn) in LAT:
                if skipq and nchk == 3:
                    continue
                for c in range(nchk):
                    P.mm(ps[:, 7, 0:n], ones(), sqr[:, r0 + c, 0:n], c == 0, c == nchk - 1)
                ri = 0
                rstd_from(rs2[:, ri, 0:n], ps[:, 7, 0:n], 128, n, invn)
                for c in range(nchk):
                    P.stt(dst[:, c, g0:g0 + n], raw[:, r0 + c, 0:n], vcol(l, gcol + c), rs2[:, ri, 0:n], ALU.mult, ALU.mult)
        P.release(*hbs, raw, sqr, rs2, sg1, w_in)
        w_out = P.alloc("w_out", [128, 4, D], BF16)
        mixcf = P.alloc("mixcf", [128, 4, TT], BF16)
        wload([w_out[:, :, :]], [w_out_d[l, 512:1024, :].rearrange("(k p) c -> p k c", p=128)], "w_out")
        dump_sb("cqn%d" % l, cqn, [128, 3, TT]); dump_sb("ckvn%d" % l, ckvn, [128, 2, TT])
        dump_sb("krope%d" % l, krope, [96, TT]); dump_sb("uT%d" % l, uT, [128, 2, TT])
        dump_sb("ypx%d" % l, ypx, [128, 2, SEQ + 30])

        cstreams = [(ypx, SEQ, 0, XBLK)] + ([] if last else [(ypc, CTX, SEQ, [(0, 256)])])
        dg = P.alloc("dg", [128, 2, 31, 128], BF16)
        for c in range(2):
            for jj in range(31):
                P.ts(dg[:, c, jj, :], identb(), vcol(l, V_CW + c * 31 + jj), ALU.mult)
        ycv = P.alloc("ycv", [128, 2, 512], F32)
        ybf = P.alloc("ybf", [128, 2, 512], BF16)
        sqy = P.alloc("sqy", [128, 2, 512], BF16)
        mu = P.alloc("mu", [128, 512], F32)
        var = P.alloc("var", [128, 512], F32)
        rsd = P.alloc("rsd", [128, 512], F32)
        dd_ = P.alloc("dd", [128, 512], F32)
        for (yp, T, goff, blks) in cstreams:
            for (b0, n) in blks:
                for c in range(2):
                    bk = 1 + c
                    for jj in range(31):
                        P.mm(ps[:, bk, 0:n], dg[:, c, jj, :], yp[:, c, b0 + jj:b0 + jj + n], jj == 0, jj == 30)
                    P.act(ycv[:, c, 0:n], ps[:, bk, 0:n], AF.Identity, bias=vcol(l, V_CB + c))
                    P.act(sqy[:, c, 0:n], ps[:, bk, 0:n], AF.Square, bias=vcol(l, V_CB + c))
                    P.copy(ybf[:, c, 0:n], ycv[:, c, 0:n], eng="pool")
                for c in range(2):
                    P.mm(ps[:, 3, 0:n], ones(), ybf[:, c, 0:n], c == 0, c == 1)
                for c in range(2):
                    P.mm(ps[:, 4, 0:n], ones(), sqy[:, c, 0:n], c == 0, c == 1)
                P.ts(mu[:, 0:n], ps[:, 3, 0:n], 1.0 / 256, ALU.mult)
                P.tt(var[:, 0:n], mu[:, 0:n], mu[:, 0:n], ALU.mult)
                P.stt(var[:, 0:n], ps[:, 4, 0:n], 1.0 / 256, var[:, 0:n], ALU.mult, ALU.subtract)
                rstd_from(rsd[:, 0:n], var[:, 0:n], 128, n, 1.0)
                for c in range(2):
                    P.tt(dd_[:, 0:n], ycv[:, c, 0:n], mu[:, 0:n], ALU.subtract)
                    P.tt(dd_[:, 0:n], dd_[:, 0:n], rsd[:, 0:n], ALU.mult)
                    P.act(mixcf[:, c, goff + b0:goff + b0 + n], dd_[:, 0:n], AF.Silu,
                          scale=vcol(l, V_LNG + c), bias=vcol(l, V_LNB + c))
        P.release(dg, ycv, ybf, sqy, mu, var, rsd, dd_, ypx, ypc)

        AB = P.alloc("AB", [128, 18, 512], BF16)
        ntc = 16 if last else 18
        for tc in range(ntc):
            bk = 1 + tc % 4
            for kc in range(2):
                P.mm(ps[:, bk, kc * 256:(kc + 1) * 256], uT[:, kc, tc * 128:(tc + 1) * 128], ccsc, True, True)
            P.copy(AB[:, tc, :], ps[:, bk, :], eng=("act" if tc % 2 else "dve"))
        P.release(uT)
        dft = [P.alloc("dft", [128, 2, 4, 1024], BF16) for _ in range(2)]
        sc_x = 1.0 / np.sqrt(SEQ * 64.0)

        def mk_dft(jn):
            half, sgp = jn // 4, jn % 4
            sl = dft[jn % 2]

            def ld():
                rows = slice(sgp * 512, (sgp + 1) * 512)
                cols = slice(half * 1024, (half + 1) * 1024)
                wload([sl[:, 0, :, :], sl[:, 1, :, :]],
                      [dftc_d[rows, cols].rearrange("(i p) c -> p i c", p=128),
                       dfts_d[rows, cols].rearrange("(i p) c -> p i c", p=128)], "dft%d" % (jn % 2), q="sp")

            def cp():
                for i in range(4):
                    scn = sgp * 4 + i
                    for c in range(2):
                        for sb in range(2):
                            bk = 1 + c * 2 + sb
                            P.mm(ps[:, bk, :], AB[:, scn, c * 256:c * 256 + 128], sl[:, 0, i, sb * 512:(sb + 1) * 512],
                                 scn == 0, False)
                            P.mm(ps[:, bk, :], AB[:, scn, c * 256 + 128:c * 256 + 256], sl[:, 1, i, sb * 512:(sb + 1) * 512],
                                 False, scn == 15)
                if sgp == 3:
                    for c in range(2):
                        for sb in range(2):
                            bk = 1 + c * 2 + sb
                            t0 = half * 1024 + sb * 512
                            P.act(mixcf[:, 2 + c, t0:t0 + 512], ps[:, bk, :], AF.Copy, scale=sc_x)
            return (ld, cp)

        run_stream([mk_dft(jn) for jn in range(8)])
        if not last:
            for c in range(2):
                bk = 5 + c
                for scn in range(2):
                    P.mm(ps[:, bk, 0:256], AB[:, 16 + scn, c * 256:c * 256 + 128], d256[:, scn, 0:256], scn == 0, False)
                    P.mm(ps[:, bk, 0:256], AB[:, 16 + scn, c * 256 + 128:c * 256 + 256], d256[:, scn, 256:512], False, scn == 1)
                P.act(mixcf[:, 2 + c, SEQ:SEQ + 256], ps[:, bk, 0:256], AF.Copy, scale=1.0 / np.sqrt(CTX * 64.0))
        P.release(AB, *dft)
        dump_sb("mixcf%d" % l, mixcf, [128, 4, TT])

        def out_proj(mixbuf, w_out):
            for (src, tb0, t0s, n, j) in streams:
                if last and j == 1:
                    continue
                g0 = tb0 + t0s
                for m in range(8):
                    bk = 1 + m % 6
                    for kk in range(4):
                        P.mm(ps[:, bk, 0:n], w_out[:, kk, m * 128:(m + 1) * 128], mixbuf[:, kk, g0:g0 + n], kk == 0, kk == 3)
                    P.stt(src[:, m, t0s:t0s + n], ps[:, bk, 0:n], mod[:, 16 + m, j:j + 1], src[:, m, t0s:t0s + n], ALU.mult, ALU.add)

        out_proj(mixcf, w_out)
        P.release(mixcf, w_out)

        mixa = P.alloc("mixa", [128, 4, TT], BF16)
        wq = P.alloc("wq", [128, 3, 768], BF16)
        wkn = P.alloc("wkn", [128, 2, 8, 96], BF16)
        wv = P.alloc("wv", [128, 2, 8, 64], BF16)
        P.memset(wkn[:, :, :, 64:96], 0.0, eng="pool")
        with nc.allow_non_contiguous_dma(reason="mla weight split"):
            ov, iv = [wq[:, :, :]], [w_uq_d[l].rearrange("(k p) c -> p k c", p=128)]
            for kc in range(2):
                src = w_ukv_d[l, kc * 128:(kc + 1) * 128, :].rearrange("p (h c) -> p h c", h=8)
                ov += [wkn[:, kc, :, 0:64], wv[:, kc, :, :]]
                iv += [src[:, :, 0:64], src[:, :, 64:128]]
            wload(ov, iv, "w_mla")
        qf = [P.alloc("qf", [96, TT], BF16) for _ in range(2)]
        kf = [P.alloc("kf", [96, TT], BF16) for _ in range(2)]
        va = [P.alloc("va", [128, 18, 128], BF16) for _ in range(2)]
        P.memset(va[0][:, :, 64:128], 1.0, eng="pool")
        P.memset(va[1][:, :, 0:64], 1.0, eng="pool")
        NSET = 3
        tset = [dict(sq1=P.alloc("sq1", [96, 512], BF16), qg=P.alloc("qg", [96, 512], BF16),
                     rs1=P.alloc("rs1", [96, 512], F32), t1=P.alloc("t1", [96, 512], F32),
                     t2=P.alloc("t2", [96, 512], F32)) for _ in range(NSET)]
        pt = [P.alloc("pt", [128, 512], BF16) for _ in range(4)]
        rec = [P.alloc("rec", [128, 512], F32) for _ in range(2)]
        Ct = lambda g0, n: rope[0:96, g0:g0 + n]
        St = lambda g0, n: rope[0:96, TT + g0:TT + g0 + n]
        scale_qk = 96.0 ** -0.5
        stepc = [0]

        def normrope_a(T, srcv, gcol, n):
            sq1, qg = T["sq1"], T["qg"]
            P.ts(qg[:, 0:n], srcv, gcol, ALU.mult)
            if srcv.sp == "PS":
                P.act(sq1[:, 0:n], srcv, AF.Square)
            else:
                P.tt(sq1[:, 0:n], srcv, srcv, ALU.mult, eng="pool")

        def normrope_b(T, g0, n, outv, pb):
            sq1, qg, rs1, t1, t2 = T["sq1"], T["qg"], T["rs1"], T["t1"], T["t2"]
            P.mm(ps[0:96, 7, 0:n], ones(96, 96), sq1[:, 0:n], True, True)
            P.mm(ps[0:96, pb, 0:n], permv, qg[:, 0:n], True, True)
            P.act(rs1[:, 0:n], ps[0:96, 7, 0:n], AF.Ln, scale=1.0 / 96, bias=epsc[0:96, 0:1])
            P.act(rs1[:, 0:n], rs1[:, 0:n], AF.Exp, scale=-0.5)
            P.tt(t1[:, 0:n], qg[:, 0:n], Ct(g0, n), ALU.mult)
            P.tt(t2[:, 0:n], ps[0:96, pb, 0:n], St(g0, n), ALU.mult)

        def normrope_c1(T, n):
            P.tt(T["t1"][:, 0:n], T["t1"][:, 0:n], T["t2"][:, 0:n], ALU.add, eng="pool")

        def normrope_c2(T, n, outv):
            P.tt(outv, T["t1"][:, 0:n], T["rs1"][:, 0:n], ALU.mult)

        allblk = [(b0, n) for (b0, n) in XBLK] + [CBLK]

        def prep_steps(h):
            hp = h % 2
            steps = []

            def kstep(g0, n):
                st = {}

                def fa():
                    T = tset[stepc[0] % NSET]; stepc[0] += 1
                    st["T"] = T
                    for kc in range(2):
                        P.mm(ps[0:96, 5, 0:n], wkn[:, kc, h, :], ckvn[:, kc, g0:g0 + n], kc == 0, kc == 1)
                    P.tt(T["t1"][:, 0:n], ps[0:96, 5, 0:n], krope[0:96, g0:g0 + n], ALU.add)
                    normrope_a(T, T["t1"][:, 0:n], vcol(l, V_KN, 1, 96), n)

                def fb():
                    normrope_b(st["T"], g0, n, kf[hp][:, g0:g0 + n], 6)
                return (fa, fb, lambda: normrope_c1(st["T"], n), lambda: normrope_c2(st["T"], n, kf[hp][:, g0:g0 + n]))

            def qstep(g0, n):
                st = {}

                def fa():
                    T = tset[stepc[0] % NSET]; stepc[0] += 1
                    st["T"] = T
                    for kc in range(3):
                        P.mm(ps[0:96, 5, 0:n], wq[:, kc, h * 96:(h + 1) * 96], cqn[:, kc, g0:g0 + n], kc == 0, kc == 2)
                    normrope_a(T, ps[0:96, 5, 0:n], vcol(l, V_QN, 1, 96), n)

                def fb():
                    normrope_b(st["T"], g0, n, qf[hp][:, g0:g0 + n], 6)
                return (fa, fb, lambda: normrope_c1(st["T"], n), lambda: normrope_c2(st["T"], n, qf[hp][:, g0:g0 + n]))

            def vstep(j0):
                def f():
                    vo = 0 if hp == 0 else 64
                    nj = min(8, 18 - j0)
                    for jj in range(nj):
                        j = j0 + jj
                        for kc in range(2):
                            P.mm(ps[:, 5, jj * 64:(jj + 1) * 64], ckvn[:, kc, j * 128:(j + 1) * 128], wv[:, kc, h, :], kc == 0, kc == 1)
                    P.copy(va[hp][:, j0:j0 + nj, vo:vo + 64],
                           ps[:, 5, 0:nj * 64].w(lambda a: a.rearrange("p (j c) -> p j c", c=64)), eng="dve")
                return (f, None, None, None)

            for (g0, n) in allblk:
                steps.append(kstep(g0, n))
            for (g0, n) in allblk:
                if last and g0 >= SEQ:
                    continue
                steps.append(qstep(g0, n))
            for j0 in (0, 8, 16):
                steps.append(vstep(j0))
            return steps

        class Prep:
            def __init__(s_, steps):
                s_.steps = list(steps)
                s_.p1 = None
                s_.p2 = None

            def tick(s_):
                c = s_.p2
                b_ = s_.p1
                nxt = s_.steps.pop(0) if s_.steps else None
                if c is not None and c[2] is not None:
                    c[2]()
                if nxt is not None:
                    nxt[0]()
                if b_ is not None and b_[1] is not None:
                    b_[1]()
                if c is not None and c[3] is not None:
                    c[3]()
                s_.p2 = b_
                s_.p1 = nxt

            def flush(s_):
                while s_.steps or s_.p1 is not None or s_.p2 is not None:
                    s_.tick()

        Prep(prep_steps(0)).flush()
        its = []
        for h in range(NH):
            qblocks = [(b0, n, list(range(18))) for (b0, n) in XBLK] + ([] if last else [(SEQ, 256, [16, 17])])
            for qi, (q0, nq, keys) in enumerate(qblocks):
                for ki, j in enumerate(keys):
                    its.append((h, qi, q0, nq, ki, j, len(keys)))
        LA = 2
        pend = {}
        for t in range(len(its) + LA):
            if t < len(its):
                h, qi, q0, nq, ki, j, nk = its[t]
                hp = h % 2
                if qi == 0 and ki == 0:
                    if h in pend:
                        pend.pop(h).flush()
                    if h + 1 < NH:
                        pend[h + 1] = Prep(prep_steps(h + 1))
                bs = 2 + (t % 3)
                P.mm(ps[:, bs, 0:nq], kf[hp][:, j * 128:(j + 1) * 128], qf[hp][:, q0:q0 + nq], True, True)
                P.act(pt[t % 4][:, 0:nq], ps[:, bs, 0:nq], AF.Exp, scale=scale_qk)
                if t % 4 == 3 and (h + 1) in pend:
                    pend[h + 1].tick()
            u = t - LA
            if u >= 0:
                h, qi, q0, nq, ki, j, nk = its[u]
                hp = h % 2
                bo = qi % 2
                P.mm(ps[:, bo, 0:nq], va[hp][:, j, :], pt[u % 4][:, 0:nq], ki == 0, ki == nk - 1)
                if ki == nk - 1:
                    no, do = (0, 64) if hp == 0 else (64, 0)
                    rc = rec[qi % 2]
                    P.recip(rc[no:no + 64, 0:nq], ps[do:do + 64, bo, 0:nq])
                    P.tt(mixa[no:no + 64, h // 2, q0:q0 + nq], ps[no:no + 64, bo, 0:nq], rc[no:no + 64, 0:nq], ALU.mult)
        tl = [v_ for T in tset for v_ in T.values()]
        P.release(wq, wkn, wv, *qf, *kf, *va, *tl, *pt, *rec, cqn, ckvn, krope)
        dump_sb("mixa%d" % l, mixa, [128, 4, TT])
        wr = None
        if not last:
            wr = [P.alloc("wr", [128, 12288], BF16) for _ in range(2)]
            wload([wr[0][:, 0:4096].w(lambda a: a.rearrange("p (k f) -> p k f", k=8)),
                   wr[0][:, 4096:8192].w(lambda a: a.rearrange("p (k f) -> p k f", k=8)),
                   wr[0][:, 8192:12288].w(lambda a: a.rearrange("p (i d) -> p i d", i=4))],
                  [fg_d[0][:, 0:512].rearrange("(k p) f -> p k f", p=128),
                   fu_d[0][:, 0:512].rearrange("(k p) f -> p k f", p=128),
                   fd_d[0][0:512, :].rearrange("(i p) d -> p i d", p=128)], "wr0")
        w_outa = P.alloc("w_outa", [128, 4, D], BF16)
        wload([w_outa[:, :, :]], [w_out_d[l, 0:512, :].rearrange("(k p) c -> p k c", p=128)], "w_outa")
        out_proj(mixa, w_outa)
        P.release(mixa, w_outa)
        dump_sb("xTm%d" % l, xT, [128, 8, SEQ])

        hT = P.alloc("hT", [128, 8, TT], BF16)
        fstreams = [s_ for s_ in streams if not (last and s_[4] == 1)]
        if last:
            h32 = P.alloc("h32", [128, 8, 512], F32)
            rw = P.alloc("rw", [128, 8, NE], F32)
            lg = P.alloc("lg", [128, 16, NE], F32)
            with nc.allow_non_contiguous_dma(reason="router"):
                P.dma("sp", rw[:, :, :], rw_d[0].rearrange("(k p) e -> p k e", p=128), P.chan("rw"))
        for (src, tb0, t0s, n, j) in fstreams:
            g0 = tb0 + t0s
            adaln_block(src, t0s, n, gsf, 24, j, hT, h32=(h32 if last else None), ho=g0)
            if last:
                for tcn in range(n // 128):
                    for dc in range(8):
                        P.mm(ps[:, 6, tcn * 8:(tcn + 1) * 8], h32[:, dc, tcn * 128:(tcn + 1) * 128], rw[:, dc, :], dc == 0, dc == 7)
                tc0 = g0 // 128
                P.copy(lg[:, tc0:tc0 + 4, :], ps[:, 6, 0:32].w(lambda a: a.rearrange("p (t e) -> p t e", e=NE)))
        if last:
            P.release(h32, rw)
            m1 = P.alloc("m1", [128, 16], F32); m2 = P.alloc("m2", [128, 16], F32)
            eq1 = P.alloc("eq1", [128, 16, NE], F32); eq2 = P.alloc("eq2", [128, 16, NE], F32)
            l2 = P.alloc("l2", [128, 16, NE], F32); gts = P.alloc("gts", [128, 16, NE], F32)
            e2 = P.alloc("e2", [128, 16], F32); w1 = P.alloc("w1", [128, 16], F32); w2 = P.alloc("w2", [128, 16], F32)
            bc = lambda v: v.w(lambda a: a.unsqueeze(2).broadcast_to([128, 16, NE]))
            AX = mybir.AxisListType.X
            P.op("dve", lambda: nc.vector.tensor_reduce(out=m1[:, :].ap, in_=lg[:, :, :].ap, axis=AX, op=ALU.max),
                 reads=[lg[:, :, :]], writes=[m1[:, :]])
            P.tt(eq1[:, :, :], lg[:, :, :], bc(m1[:, :]), ALU.is_equal)
            P.stt(l2[:, :, :], eq1[:, :, :], -1e30, lg[:, :, :], ALU.mult, ALU.add)
            P.op("dve", lambda: nc.vector.tensor_reduce(out=m2[:, :].ap, in_=l2[:, :, :].ap, axis=AX, op=ALU.max),
                 reads=[l2[:, :, :]], writes=[m2[:, :]])
            P.tt(eq2[:, :, :], l2[:, :, :], bc(m2[:, :]), ALU.is_equal)
            P.tt(e2[:, :], m2[:, :], m1[:, :], ALU.subtract)
            P.act(e2[:, :], e2[:, :], AF.Exp)
            P.ts(w1[:, :], e2[:, :], 1.0, ALU.add)
            P.recip(w1[:, :], w1[:, :])
            P.tt(w2[:, :], e2[:, :], w1[:, :], ALU.mult)
            P.tt(gts[:, :, :], eq1[:, :, :], bc(w1[:, :]), ALU.mult)
            P.tt(eq2[:, :, :], eq2[:, :, :], bc(w2[:, :]), ALU.mult)
            P.tt(gts[:, :, :], gts[:, :, :], eq2[:, :, :], ALU.add)
            gT = P.alloc("gT", [8, SEQ], F32)
            selb = P.alloc("selb", [8, 1024], F32)
            P.dma("sp", selb[:, :], sel_d, P.chan("selb"))
            for tcn in range(16):
                bk = 1 + (tcn // 4) % 2
                P.transpose(ps[0:8, bk, (tcn % 4) * 128:(tcn % 4 + 1) * 128], gts[:, tcn, :], identf[:, :])
                if tcn % 4 == 3:
                    P.copy(gT[:, (tcn - 3) * 128:(tcn + 1) * 128], ps[0:8, bk, :])
            dump_sb("gT", gT, [8, SEQ])
            P.release(m1, m2, eq1, eq2, l2, e2, w1, w2, lg, gts)
            gb = P.alloc("gb", [128, SEQ], F32)

        pre0 = wr is not None
        if wr is None:
            wr = [P.alloc("wr", [128, 12288], BF16) for _ in range(2)]
        aT = [P.alloc("aT", [128, 4, 512], BF16) for _ in range(2)]
        sgt = [P.alloc("sgt", [128, 512], F32) for _ in range(2)]
        tmu = [P.alloc("tmu", [128, 512], F32) for _ in range(2)]
        groups = [(0, 4), (4, 4), (8, 4), (12, 4), (16, 4), (20, 2)]
        experts = [None] if not last else list(range(NE))
        jobs = []

        def mk_ffn(jobn, e, c0, ncg, first_of_expert):
            sl = wr[jobn % 2]
            f0 = c0 * 128
            nf = ncg * 128
            if e is None:
                Wg, Wu, Wd = fg_d[0], fu_d[0], fd_d[0]
            else:
                Wg, Wu, Wd = mg_d[0, e], mu_d[0, e], md_d[0, e]
            wgv = sl[:, 0:8 * nf].w(lambda a: a.rearrange("p (k f) -> p k f", k=8))
            wuv = sl[:, 4096:4096 + 8 * nf].w(lambda a: a.rearrange("p (k f) -> p k f", k=8))
            wdv = sl[:, 8192:8192 + ncg * 1024].w(lambda a: a.rearrange("p (i d) -> p i d", i=ncg))

            def ld():
                if jobn == 0 and pre0:
                    return
                wload([wgv, wuv, wdv],
                      [Wg[:, f0:f0 + nf].rearrange("(k p) f -> p k f", p=128),
                       Wu[:, f0:f0 + nf].rearrange("(k p) f -> p k f", p=128),
                       Wd[f0:f0 + nf, :].rearrange("(i p) d -> p i d", p=128)], "wr%d" % (jobn % 2))

            def cp():
                if e is not None and first_of_expert:
                    for bi, (b0, n) in enumerate(XBLK):
                        bk = 6 + bi % 2
                        P.mm(ps[:, bk, 0:n], selb[0:8, e * 128:(e + 1) * 128], gT[0:8, b0:b0 + n], True, True)
                        P.copy(gb[:, b0:b0 + n], ps[:, bk, 0:n], eng="act")
                wg3 = lambda dc, i: V(wgv.ap[:, dc, i * 128:(i + 1) * 128], wgv.sp, wgv.lo, wgv.hi)
                wu3 = lambda dc, i: V(wuv.ap[:, dc, i * 128:(i + 1) * 128], wuv.sp, wuv.lo, wuv.hi)
                wd3 = lambda i, m: V(wdv.ap[:, i, m * 128:(m + 1) * 128], wdv.sp, wdv.lo, wdv.hi)
                def gu_part(bi):
                    (src, tb0, t0s, n, j) = fstreams[bi]
                    g0 = tb0 + t0s
                    a_ = aT[bi % 2]
                    for i in range(ncg):
                        bg, bu = (0, 1) if i % 2 == 0 else (2, 3)
                        for dc in range(8):
                            P.mm(ps[:, bg, 0:n], wg3(dc, i), hT[:, dc, g0:g0 + n], dc == 0, dc == 7)
                        for dc in range(8):
                            P.mm(ps[:, bu, 0:n], wu3(dc, i), hT[:, dc, g0:g0 + n], dc == 0, dc == 7)
                        s_ = sgt[i % 2]
                        P.act(s_[:, 0:n], ps[:, bg, 0:n], AF.Silu)
                        if e is None:
                            P.tt(a_[:, i, 0:n], s_[:, 0:n], ps[:, bu, 0:n], ALU.mult)
                        else:
                            u_ = tmu[i % 2]
                            P.tt(u_[:, 0:n], ps[:, bu, 0:n], gb[:, g0:g0 + n], ALU.mult)
                            P.tt(a_[:, i, 0:n], s_[:, 0:n], u_[:, 0:n], ALU.mult, eng="pool")

                def down_part(bi):
                    (src, tb0, t0s, n, j) = fstreams[bi]
                    a_ = aT[bi % 2]
                    for m in range(8):
                        bk = 4 + m % 4
                        for i in range(ncg):
                            P.mm(ps[:, bk, 0:n], wd3(i, m), a_[:, i, 0:n], i == 0, i == ncg - 1)
                        P.stt(src[:, m, t0s:t0s + n], ps[:, bk, 0:n], mod[:, 40 + m, j:j + 1], src[:, m, t0s:t0s + n], ALU.mult, ALU.add)

                nb_ = len(fstreams)
                for bi in range(nb_ + 1):
                    if bi < nb_:
                        gu_part(bi)
                    if bi >= 1:
                        down_part(bi - 1)
            return (ld, cp)

        jn = 0
        for e in experts:
            for gi, (c0, ncg) in enumerate(groups):
                jobs.append(mk_ffn(jn, e, c0, ncg, gi == 0))
                jn += 1
        run_stream(jobs)
        P.release(hT, *wr, *aT, *sgt, *tmu)
        if last:
            P.release(gT, selb, gb)
        dump_sb("xTf%d" % l, xT, [128, 8, SEQ])

    osb = [P.alloc("osb", [128, D], F32) for _ in range(2)]
    co = [P.chan("o0"), P.chan("o1")]
    for i in range(16):
        ob = osb[i % 2]
        for half in range(2):
            bk = (2 * i + half) % 8
            for q in range(4):
                dc = half * 4 + q
                P.transpose(ps[:, bk, q * 128:(q + 1) * 128], xT[:, dc, i * 128:(i + 1) * 128], identf[:, :])
            P.copy(ob[:, half * 512:(half + 1) * 512], ps[:, bk, :], eng=("act" if half else "dve"))
        P.dma("sp", out_d[i * 128:(i + 1) * 128, :], ob[:, :], co[i % 2])
    fin = P.alloc("fin", [128, 16], F32)
    nw = P.emit()
    for ch in co + [c_ for n_, c_ in P.chans.items() if n_.startswith("out_")]:
        nc.sync.wait_ge(ch.sem, ch.count)
    return nc, dumps, len(P.ins), nw


_BUILT = {}


def make_inputs(inputs, b):
    c = consts()
    m = {"x": np.ascontiguousarray(inputs["x"][b], dtype=np.float32),
         "ctx": np.ascontiguousarray(inputs["ctx"][b], dtype=np.float32),
         "vecs": pack_vecs(inputs, b)}
    m.update(c)
    for k in ("ada_w", "w_in", "w_uq", "w_ukv", "w_out", "ffn_w_gate", "ffn_w_up", "ffn_w_down", "router_w",
              "moe_w_gate", "moe_w_up", "moe_w_down"):
        m[k] = np.ascontiguousarray(inputs[k], dtype=np.float32)
    return m


def kernel(**inputs):
    inputs = {k: np.asarray(v) for k, v in inputs.items()}
    if "nc" not in _BUILT:
        _BUILT["nc"] = build()
    nc = _BUILT["nc"][0]
    in_maps = [make_inputs(inputs, b) for b in range(8)]
    res = run_bass_kernel_spmd(nc, in_maps, core_ids=list(range(8)))
    return np.stack([np.asarray(r["out"], dtype=np.float32) for r in res.results], axis=0)
```

```python
import numpy as np
import ml_dtypes
from bisect import bisect_left
import concourse.bass as bass
import concourse.mybir as mybir
from concourse.bass_utils import run_bass_kernel_spmd

F32 = mybir.dt.float32
BF16 = mybir.dt.bfloat16
AF = mybir.ActivationFunctionType
ALU = mybir.AluOpType
ES = {F32: 4, BF16: 2}

D = 1024
SEQ = 2048
CTX = 256
TT = SEQ + CTX
NH = 8
DFF = 2816
NE = 8
EPS = 1e-6
SB_LO = 16512
SB_HI = 229344


class V:
    __slots__ = ("ap", "sp", "lo", "hi")

    def __init__(s, ap, sp, lo, hi):
        s.ap, s.sp, s.lo, s.hi = ap, sp, lo, hi

    def w(s, fn):
        return V(fn(s.ap), s.sp, s.lo, s.hi)


class Tn:
    def __init__(s, t, shape, dtype, space, base):
        s.t, s.shape, s.dtype, s.space, s.base = t, list(shape), dtype, space, base
        s.es = ES[dtype]
        st = [1] * len(shape)
        for i in range(len(shape) - 2, 0, -1):
            st[i] = st[i + 1] * shape[i + 1]
        s.st = st
        s.nbytes = int(np.prod(shape[1:])) * s.es

    def __getitem__(s, idx):
        if not isinstance(idx, tuple):
            idx = (idx,)
        ap = s.t[idx]
        lo = 0
        hi = 0
        for d in range(1, len(s.shape)):
            n = s.shape[d]
            if d < len(idx):
                ix = idx[d]
                if isinstance(ix, slice):
                    a, b, c = ix.indices(n)
                    last = a + ((b - a - 1) // c) * c
                else:
                    a = ix
                    last = ix
            else:
                a, last = 0, n - 1
            lo += a * s.st[d]
            hi += last * s.st[d]
        lo_b = s.base + lo * s.es
        hi_b = s.base + (hi + 1) * s.es
        if s.space == "PS":
            lo_b = (lo_b // 2048) * 2048
            hi_b = ((hi_b + 2047) // 2048) * 2048
        return V(ap, s.space, lo_b, hi_b)


class Segs:
    def __init__(s):
        s.b = [0, 1 << 40]
        s.w = [None]
        s.r = [{}]

    def _split(s, x):
        i = bisect_left(s.b, x)
        if s.b[i] == x:
            return
        s.b.insert(i, x)
        s.w.insert(i, s.w[i - 1])
        s.r.insert(i, dict(s.r[i - 1]))

    def access(s, lo, hi, idx, key, write, deps):
        s._split(lo)
        s._split(hi)
        i = bisect_left(s.b, lo)
        while s.b[i] < hi:
            if s.w[i] is not None:
                deps.add(s.w[i])
            if write:
                for v in s.r[i].values():
                    deps.add(v)
                s.w[i] = idx
                s.r[i] = {}
            else:
                s.r[i][key] = idx
            i += 1


class Chan:
    def __init__(s, sem, bulk, name):
        s.sem, s.bulk, s.count, s.name = sem, bulk, 0, name


class Ins:
    __slots__ = ("eng", "fn", "deps", "chan", "cval", "signal", "sigval")


class Prog:
    def __init__(s, nc):
        s.nc = nc
        s.E = {"pe": nc.tensor, "act": nc.scalar, "dve": nc.vector, "pool": nc.gpsimd, "sp": nc.sync}
        s.ins = []
        s.segs = {"SB": Segs(), "PS": Segs()}
        s.sems = {e: nc.alloc_semaphore("s_" + e) for e in ("pe", "act", "dve", "pool")}
        s.chans = {}
        s.free = [(SB_LO, SB_HI)]
        s.nalloc = 0
        s.ps_t = nc.alloc_psum_tensor("psall", [128, 8, 512], F32)
        s.ps = Tn(s.ps_t, [128, 8, 512], F32, "PS", 0)

    def alloc(s, name, shape, dtype):
        nb = int(np.prod(shape[1:])) * ES[dtype]
        nb = (nb + 63) // 64 * 64
        for i, (a, b) in enumerate(s.free):
            if b - a >= nb:
                s.free[i] = (a + nb, b)
                if s.free[i][0] == s.free[i][1]:
                    s.free.pop(i)
                s.nalloc += 1
                t = s.nc.alloc_sbuf_tensor_at("%s_%d" % (name, s.nalloc), list(shape), dtype, offset=a)
                tn = Tn(t, shape, dtype, "SB", a)
                tn.size = nb
                return tn
        raise RuntimeError("SBUF alloc failed for %s (%d B); free=%s" % (name, nb, s.free))

    def release(s, *tns):
        for tn in tns:
            s.free.append((tn.base, tn.base + tn.size))
        s.free.sort()
        m = []
        for a, b in s.free:
            if m and m[-1][1] == a:
                m[-1] = (m[-1][0], b)
            else:
                m.append((a, b))
        s.free = m

    def chan(s, name, bulk=False):
        if name not in s.chans:
            s.chans[name] = Chan(s.nc.alloc_semaphore("c_" + name), bulk, name)
        return s.chans[name]

    def op(s, eng, fn, reads=(), writes=(), chan=None):
        ins = Ins()
        ins.eng, ins.fn, ins.chan, ins.signal, ins.sigval, ins.cval = eng, fn, chan, False, 0, 0
        idx = len(s.ins)
        deps = set()
        key = eng if chan is None else "c:" + chan.name
        for v in reads:
            if isinstance(v, V):
                s.segs[v.sp].access(v.lo, v.hi, idx, key, v.sp == "PS", deps)
        for v in writes:
            if isinstance(v, V):
                s.segs[v.sp].access(v.lo, v.hi, idx, key, True, deps)
        if chan is not None:
            chan.count += 16
            ins.cval = chan.count
        deps.discard(idx)
        ins.deps = deps
        s.ins.append(ins)
        return idx

    def emit(s):
        ins = s.ins
        need = []
        for i, x in enumerate(ins):
            nd = {}
            for j in x.deps:
                y = ins[j]
                if y.chan is not None:
                    k = ("c", y.chan.name)
                    nd[k] = max(nd.get(k, -1), j)
                else:
                    if y.eng == "pe" and x.eng == "pe" and x.chan is None:
                        continue
                    k = ("e", y.eng)
                    nd[k] = max(nd.get(k, -1), j)
            need.append(nd)
            for k, j in nd.items():
                if k[0] == "e":
                    ins[j].signal = True
        cnt = {e: 0 for e in s.sems}
        for x in ins:
            if x.signal:
                cnt[x.eng] += 1
                x.sigval = cnt[x.eng]
        waited = {e: {} for e in s.E}
        nw = 0
        for i, x in enumerate(ins):
            eng = s.E[x.eng]
            wd = waited[x.eng]
            for k, j in need[i].items():
                y = ins[j]
                if k[0] == "c":
                    sem = y.chan.sem
                    val = y.chan.count if y.chan.bulk else y.cval
                else:
                    sem = s.sems[y.eng]
                    val = y.sigval
                sid = id(sem)
                if wd.get(sid, 0) >= val:
                    continue
                wd[sid] = val
                eng.wait_ge(sem, val)
                nw += 1
            bi = x.fn()
            if x.chan is not None:
                bi.then_inc(x.chan.sem, 16)
            elif x.signal:
                bi.then_inc(s.sems[x.eng], 1)
        return nw

    def mm(s, out, lhsT, rhs, start, stop):
        nc = s.nc
        return s.op("pe", lambda: nc.tensor.matmul(out.ap, lhsT=lhsT.ap, rhs=rhs.ap, start=start, stop=stop),
                    reads=[lhsT, rhs], writes=[out])

    def transpose(s, out, in_, ident):
        nc = s.nc
        return s.op("pe", lambda: nc.tensor.transpose(out.ap, in_.ap, ident.ap), reads=[in_, ident], writes=[out])

    def act(s, out, in_, func, scale=None, bias=None):
        nc = s.nc
        kw = {}
        rd = [in_]
        if scale is not None:
            if isinstance(scale, V):
                kw["scale"] = scale.ap
                rd.append(scale)
            else:
                kw["scale"] = float(scale)
        if bias is not None:
            if isinstance(bias, V):
                kw["bias"] = bias.ap
                rd.append(bias)
            else:
                kw["bias"] = float(bias)
        return s.op("act", lambda: nc.scalar.activation(out=out.ap, in_=in_.ap, func=func, **kw), reads=rd, writes=[out])

    def tt(s, out, a, b, op, eng="dve"):
        e = s.E[eng]
        return s.op(eng, lambda: e.tensor_tensor(out=out.ap, in0=a.ap, in1=b.ap, op=op), reads=[a, b], writes=[out])

    def ts(s, out, a, s1, op0, s2=None, op1=None, eng="dve"):
        e = s.E[eng]
        rd = [a]
        a1 = s1.ap if isinstance(s1, V) else float(s1)
        if isinstance(s1, V):
            rd.append(s1)
        if op1 is None:
            return s.op(eng, lambda: e.tensor_scalar(out=out.ap, in0=a.ap, scalar1=a1, scalar2=None, op0=op0),
                        reads=rd, writes=[out])
        a2 = s2.ap if isinstance(s2, V) else float(s2)
        if isinstance(s2, V):
            rd.append(s2)
        return s.op(eng, lambda: e.tensor_scalar(out=out.ap, in0=a.ap, scalar1=a1, scalar2=a2, op0=op0, op1=op1),
                    reads=rd, writes=[out])

    def stt(s, out, a, sc, b, op0, op1):
        nc = s.nc
        rd = [a, b]
        a1 = sc.ap if isinstance(sc, V) else float(sc)
        if isinstance(sc, V):
            rd.append(sc)
        return s.op("dve", lambda: nc.vector.scalar_tensor_tensor(out=out.ap, in0=a.ap, scalar=a1, in1=b.ap, op0=op0, op1=op1),
                    reads=rd, writes=[out])

    def copy(s, out, in_, eng="dve"):
        if eng == "act":
            return s.act(out, in_, AF.Copy)
        e = s.E[eng]
        return s.op(eng, lambda: e.tensor_copy(out=out.ap, in_=in_.ap), reads=[in_], writes=[out])

    def recip(s, out, in_):
        nc = s.nc
        return s.op("dve", lambda: nc.vector.reciprocal(out=out.ap, in_=in_.ap), reads=[in_], writes=[out])

    def memset(s, out, val, eng="dve"):
        e = s.E[eng]
        return s.op(eng, lambda: e.memset(out.ap, val), writes=[out])

    def dma(s, q, out, in_, chan):
        e = s.E[q]
        oa = out.ap if isinstance(out, V) else out
        ia = in_.ap if isinstance(in_, V) else in_
        return s.op(q, lambda: e.dma_start(out=oa, in_=ia), reads=[in_], writes=[out], chan=chan)


def _col(v, n):
    v = np.asarray(v, np.float32).reshape(-1)
    pad = n * 128 - v.size
    if pad:
        v = np.concatenate([v, np.zeros(pad, np.float32)])
    return v.reshape(n, 128).T


VL = 139
V_MIXG, V_FFNG, V_ADAB, V_QLAT, V_KVLAT, V_QN, V_KN, V_CB, V_LNG, V_LNB, V_CW = 0, 8, 16, 64, 67, 69, 70, 71, 73, 75, 77


def pack_vecs(inp, b):
    cols = [_col(inp["c"][b], 8), _col(inp["c_ctx"], 8)]
    for l in range(2):
        cols += [_col(inp["mix_norm_g"][l], 8), _col(inp["ffn_norm_g"][l], 8), _col(inp["ada_b"][l], 48),
                 _col(inp["q_lat_g"][l], 3), _col(inp["kv_lat_g"][l], 2), _col(inp["q_norm_g"][l], 1),
                 _col(inp["k_norm_g"][l], 1), _col(inp["conv_b"][l], 2), _col(inp["conv_ln_g"][l], 2),
                 _col(inp["conv_ln_b"][l], 2)]
        cw = np.asarray(inp["conv_w"][l], np.float32)
        cols += [cw[:, 0:128].T, cw[:, 128:256].T]
    return np.ascontiguousarray(np.concatenate(cols, axis=1), dtype=np.float32)


_CONST = {}


def consts():
    if _CONST:
        return _CONST
    bf = ml_dtypes.bfloat16
    ident = np.eye(128, dtype=np.float32)
    perm = np.zeros((128, 96), np.float32)
    for i in range(16):
        perm[80 + i, 64 + i] = -1.0
        perm[64 + i, 80 + i] = 1.0
    k = np.arange(64)
    ang = 2 * np.pi * np.outer(k, k) / 64.0
    cc = np.zeros((128, 128)); scn = np.zeros((128, 128))
    for g in range(2):
        cc[g * 64:(g + 1) * 64, g * 64:(g + 1) * 64] = np.cos(ang)
        scn[g * 64:(g + 1) * 64, g * 64:(g + 1) * 64] = -np.sin(ang)
    cb = np.concatenate([ident, np.ones((128, 128)), perm, cc, scn], axis=1).astype(bf)
    t = np.arange(SEQ)
    row = (t // 64).astype(np.float64); col = (t % 64).astype(np.float64)
    inv = 10000.0 ** (-np.arange(8, dtype=np.float64) / 8)
    a = np.concatenate([row[:, None] * inv, col[:, None] * inv], axis=1)
    C = np.ones((96, TT)); S = np.zeros((96, TT))
    C[64:80, :SEQ] = np.cos(a).T; C[80:96, :SEQ] = np.cos(a).T
    S[64:80, :SEQ] = np.sin(a).T; S[80:96, :SEQ] = np.sin(a).T
    rope = np.concatenate([C, S], axis=1).astype(bf)
    s = np.arange(SEQ, dtype=np.float64)
    m = np.outer(s, s) % SEQ
    dc = np.cos(2 * np.pi * m / SEQ).astype(bf)
    ds = np.sin(2 * np.pi * m / SEQ).astype(bf)
    s2 = np.arange(CTX, dtype=np.float64)
    m2 = np.outer(s2, s2) % CTX
    d256 = np.concatenate([np.cos(2 * np.pi * m2 / CTX), np.sin(2 * np.pi * m2 / CTX)], axis=1).astype(bf)
    sel = np.zeros((8, 8, 128), np.float32)
    for e in range(8):
        sel[e, e, :] = 1.0
    _CONST.update(dict(identf=ident, cb=cb, rope=rope, dftc=dc, dfts=ds, dft256=d256, sel=sel.reshape(8, 1024)))
    return _CONST


XBLK = [(0, 512), (512, 512), (1024, 512), (1536, 512)]
CBLK = (2048, 256)


def build(dump=None, nlayers=2, moe_dense=True):
    nc = bass.Bass("TRN2", target_bir_lowering=False)
    P = Prog(nc)

    def din(name, shape, dt=F32):
        return nc.dram_tensor(name, list(shape), dt, kind="ExternalInput").ap()

    x_d = din("x", [SEQ, D]); ctx_d = din("ctx", [CTX, D]); vecs_d = din("vecs", [128, 16 + 2 * VL])
    identf_d = din("identf", [128, 128]); cb_d = din("cb", [128, 608], BF16); rope_d = din("rope", [96, 2 * TT], BF16)
    dftc_d = din("dftc", [SEQ, SEQ], BF16); dfts_d = din("dfts", [SEQ, SEQ], BF16); d256_d = din("dft256", [CTX, 512], BF16)
    sel_d = din("sel", [8, 1024])
    ada_w_d = din("ada_w", [2, D, 6 * D]); w_in_d = din("w_in", [2, D, 1440]); w_uq_d = din("w_uq", [2, 384, 768])
    w_ukv_d = din("w_ukv", [2, 256, 1024]); w_out_d = din("w_out", [2, D, D])
    fg_d = din("ffn_w_gate", [1, D, DFF]); fu_d = din("ffn_w_up", [1, D, DFF]); fd_d = din("ffn_w_down", [1, DFF, D])
    rw_d = din("router_w", [1, D, NE])
    mg_d = din("moe_w_gate", [1, NE, D, DFF]); mu_d = din("moe_w_up", [1, NE, D, DFF]); md_d = din("moe_w_down", [1, NE, DFF, D])
    out_d = nc.dram_tensor("out", [SEQ, D], F32, kind="ExternalOutput").ap()
    dumps = {}

    ps = P.ps
    cconst = P.chan("const", bulk=True)

    xT = P.alloc("xT", [128, 8, SEQ], F32)
    cT = P.alloc("cT", [128, 8, CTX], F32)
    vecs = P.alloc("vecs", [128, 16 + 2 * VL], F32)
    identf = P.alloc("identf", [128, 128], F32)
    cb = P.alloc("cb", [128, 608], BF16)
    rope = P.alloc("rope", [96, 2 * TT], BF16)
    d256 = P.alloc("d256", [128, 2, 512], BF16)
    sc2 = P.alloc("sc2", [128, 8, 2], BF16)
    mod = P.alloc("mod", [128, 48, 2], F32)
    gsm = P.alloc("gsm", [128, 8, 2], F32)
    gsf = P.alloc("gsf", [128, 8, 2], F32)
    P.dma("sp", vecs[:, :], vecs_d, cconst)
    P.dma("sp", identf[:, :], identf_d, cconst)
    P.dma("sp", cb[:, :], cb_d, cconst)
    P.dma("sp", rope[:, :], rope_d, cconst)
    P.dma("sp", d256[:, :, :], d256_d.rearrange("(i p) c -> p i c", p=128), cconst)
    identb = lambda a=128, b=128: cb[0:a, 0:b]
    ones = lambda a=128, b=128: cb[0:a, 128:128 + b]
    permv = cb[0:96, 256:352]
    ccsc = cb[:, 352:608]

    def vcol(l, off, n=1, rows=128):
        c0 = 16 + l * VL + off
        return vecs[0:rows, c0:c0 + n]

    xin = [P.alloc("xin", [128, D], F32) for _ in range(2)]
    cx = [P.chan("xin0"), P.chan("xin1")]
    tiles = [(x_d, i, xT, i * 128) for i in range(16)] + [(ctx_d, i, cT, i * 128) for i in range(2)]
    for n, (src, i, dst, t0) in enumerate(tiles):
        xb = xin[n % 2]
        P.dma("sp", xb[:, :], src[i * 128:(i + 1) * 128, :], cx[n % 2])
        for half in range(2):
            bk = (2 * n + half) % 8
            for q in range(4):
                dc = half * 4 + q
                P.transpose(ps[:, bk, q * 128:(q + 1) * 128], xb[:, dc * 128:(dc + 1) * 128], identf[:, :])
            src_v = ps[:, bk, :].w(lambda a: a.rearrange("p (q t) -> p q t", q=4))
            P.copy(dst[:, half * 4:half * 4 + 4, t0:t0 + 128], src_v, eng=("act" if half else "dve"))
    P.release(*xin)
    P.act(sc2[:, :, 0], vecs[:, 0:8], AF.Silu)
    P.act(sc2[:, :, 1], vecs[:, 8:16], AF.Silu)


    def wload(slot_views, srcs, chname, q="pool"):
        ch = P.chan(chname)
        ids = [P.dma(q, v, sap, ch) for v, sap in zip(slot_views, srcs)]
        for i_ in ids:
            P.ins[i_].cval = ch.count
            P.ins[i_].deps -= set(ids)

    def run_stream(jobs):
        if jobs:
            jobs[0][0]()
        for n_ in range(len(jobs)):
            if n_ + 1 < len(jobs):
                jobs[n_ + 1][0]()
            jobs[n_][1]()

    def adaln_block(src, t0s, n, gs, shoff, j, hout, h32=None, ho=0):
        sq = P.alloc("sq", [128, 3, 512], BF16)
        rt = P.alloc("rt", [128, 512], F32)
        rstd = P.alloc("rstd", [128, 512], F32)
        tmp = [P.alloc("tmp", [128, 512], F32) for _ in range(2)]
        bk = 7
        for dc in range(8):
            P.act(sq[:, dc % 3, 0:n], src[:, dc, t0s:t0s + n], AF.Square)
            P.mm(ps[:, bk, 0:n], ones(), sq[:, dc % 3, 0:n], dc == 0, dc == 7)
        rsqrt_to(rstd[:, 0:n], ps[:, bk, 0:n], 128, n, 1.0 / D, rt[:, 0:n])
        for dc in range(8):
            t = tmp[dc % 2] if h32 is None else None
            tv = t[:, 0:n] if h32 is None else h32[:, dc, 0:n]
            P.stt(tv, src[:, dc, t0s:t0s + n], gs[:, dc, j:j + 1], rstd[:, 0:n], ALU.mult, ALU.mult)
            if h32 is None:
                P.act(hout[:, dc, ho:ho + n], tv, AF.Identity, bias=mod[:, shoff + dc, j:j + 1])
            else:
                P.ts(tv, tv, mod[:, shoff + dc, j:j + 1], ALU.add)
                P.copy(hout[:, dc, ho:ho + n], tv, eng="act")
        P.release(sq, rt, rstd, *tmp)

    epsc = P.alloc("epsc", [128, 1], F32)
    P.memset(epsc[:, :], EPS)

    def rsqrt_to(out, ssum, npart, n, inv_n, tmp):
        P.act(tmp, ssum, AF.Ln, scale=inv_n, bias=epsc[0:npart, 0:1])
        P.act(out, tmp, AF.Exp, scale=-0.5)

    def rstd_from(out, ssum, npart, n, inv_n):
        rt = P.alloc("rt2", [128, 512], F32)
        rsqrt_to(out, ssum, npart, n, inv_n, rt[0:npart, 0:n])
        P.release(rt)

    def dump_sb(name, tn, shape):
        if dump is None or name not in dump:
            return
        dd = nc.dram_tensor("dbg_" + name, list(shape), tn.dtype, kind="ExternalOutput").ap()
        dumps[name] = dd
        idx = tuple(slice(0, n) for n in shape)
        P.dma("sp", dd, tn[idx], P.chan("out_" + name))

    dump_sb("xT0", xT, [128, 8, SEQ])

    for l in range(nlayers):
        last = (l == 1)
        streams = [(xT, 0, b0, n, 0) for (b0, n) in XBLK] + [(cT, 2048, 0, 256, 1)]
        adab = [P.alloc("adab", [128, 8, 768], BF16) for _ in range(2)]

        def mk_ada(p):
            sl = adab[p % 2]

            def ld():
                wload([sl[:, :, :]], [ada_w_d[l, :, p * 768:(p + 1) * 768].rearrange("(k p) c -> p k c", p=128)], "adab%d" % (p % 2))

            def cp():
                for cc in range(6):
                    g = p * 6 + cc
                    for k in range(8):
                        P.mm(ps[:, 0, 2 * g:2 * g + 2], sl[:, k, cc * 128:(cc + 1) * 128], sc2[:, k, :], k == 0, k == 7)
            return (ld, cp)

        run_stream([mk_ada(p) for p in range(8)])
        for j in range(2):
            P.tt(mod[:, :, j], ps[:, 0, j:96:2], vcol(l, V_ADAB, 48), ALU.add)
        for j in range(2):
            P.stt(gsm[:, :, j], mod[:, 8:16, j], 1.0, vcol(l, V_MIXG, 8), ALU.add, ALU.mult)
            P.stt(gsf[:, :, j], mod[:, 32:40, j], 1.0, vcol(l, V_FFNG, 8), ALU.add, ALU.mult)
        P.release(*adab)
        dump_sb("mod%d" % l, mod, [128, 48, 2])

        cqn = P.alloc("cqn", [128, 3, TT], BF16)
        ckvn = P.alloc("ckvn", [128, 2, TT], BF16)
        krope = P.alloc("krope", [128, TT], F32)
        uT = P.alloc("uT", [128, 2, TT], BF16)
        ypx = P.alloc("ypx", [128, 2, SEQ + 30], BF16)
        ypc = P.alloc("ypc", [128, 2, CTX + 30], BF16)
        w_in = P.alloc("w_in", [128, 8, 1536], BF16)
        P.memset(w_in[:, :, 1440:1504], 0.0, eng="pool")
        with nc.allow_non_contiguous_dma(reason="kr cols"):
            wload([w_in[:, :, 0:1440], w_in[:, :, 1504:1536]],
                  [w_in_d[l].rearrange("(k p) c -> p k c", p=128),
                   w_in_d[l, :, 640:672].rearrange("(k p) c -> p k c", p=128)], "w_in")
        for yp, T in ((ypx, SEQ), (ypc, CTX)):
            P.memset(yp[:, :, 0:15], 0.0, eng="pool")
            P.memset(yp[:, :, T + 15:T + 30], 0.0, eng="pool")
        hbs = [P.alloc("hb", [128, 8, 512], BF16) for _ in range(2)]
        raw = P.alloc("raw", [128, 5, 512], F32)
        sqr = P.alloc("sqr", [128, 5, 512], BF16)
        rs2 = P.alloc("rs2", [128, 1, 512], F32)
        sg1 = P.alloc("sg", [128, 512], F32)
        sg = [sg1, sg1]
        bkc = [0]

        def nb():
            bkc[0] = (bkc[0] + 1) % 6
            return 1 + bkc[0]

        LAT = ((3, 0, 0, V_QLAT, cqn, 1.0 / 384), (2, 384, 3, V_KVLAT, ckvn, 1.0 / 256))
        for si, (src, tb0, t0s, n, j) in enumerate(streams):
            hb = hbs[si % 2]
            adaln_block(src, t0s, n, gsm, 0, j, hb)
            g0 = tb0 + t0s
            skipq = last and j == 1

            def proj(c0, M):
                bk = nb()
                for dc in range(8):
                    P.mm(ps[0:M, bk, 0:n], w_in[:, dc, c0:c0 + M], hb[:, dc, 0:n], dc == 0, dc == 7)
                return bk

            for (nchk, c00, r0, gcol, dst, invn) in LAT:
                if skipq and nchk == 3:
                    continue
                for c in range(nchk):
                    bk = proj(c00 + c * 128, 128)
                    P.act(sqr[:, r0 + c, 0:n], ps[:, bk, 0:n], AF.Square)
                    P.copy(raw[:, r0 + c, 0:n], ps[:, bk, 0:n], eng="dve")
            bk = proj(1440, 96)
            P.copy(krope[0:96, g0:g0 + n], ps[0:96, bk, 0:n], eng="act")
            if not skipq:
                yp = ypx if j == 0 else ypc
                for c in range(2):
                    bg = proj(928 + c * 128, 128)
                    ba = proj(672 + c * 128, 128)
                    P.act(sg[c][:, 0:n], ps[:, bg, 0:n], AF.Sigmoid)
                    P.tt(yp[:, c, 15 + t0s:15 + t0s + n], ps[:, ba, 0:n], sg[c][:, 0:n], ALU.mult)
                for c in range(2):
                    bk = proj(1184 + c * 128, 128)
                    P.copy(uT[:, c, g0:g0 + n], ps[:, bk, 0:n], eng="act")
            for (nchk, c00, r0, gcol, dst, invn) in LAT:
                if skipq and nchk == 3:
                    continue
                for c in range(nchk):
                    P.mm(ps[:, 7, 0:n], ones(), sqr[:, r0 + c, 0:n], c == 0, c == nchk - 1)
                ri = 0
                rstd_from(rs2[:, ri, 0:n], ps[:, 7, 0:n], 128, n, invn)
                for c in range(nchk):
                    P.stt(dst[:, c, g0:g0 + n], raw[:, r0 + c, 0:n], vcol(l, gcol + c), rs2[:, ri, 0:n], ALU.mult, ALU.mult)
        P.release(*hbs, raw, sqr, rs2, sg1, w_in)
        w_out = P.alloc("w_out", [128, 4, D], BF16)
        mixcf = P.alloc("mixcf", [128, 4, TT], BF16)
        wload([w_out[:, :, :]], [w_out_d[l, 512:1024, :].rearrange("(k p) c -> p k c", p=128)], "w_out")
        dump_sb("cqn%d" % l, cqn, [128, 3, TT]); dump_sb("ckvn%d" % l, ckvn, [128, 2, TT])
        dump_sb("krope%d" % l, krope, [96, TT]); dump_sb("uT%d" % l, uT, [128, 2, TT])
        dump_sb("ypx%d" % l, ypx, [128, 2, SEQ + 30])

        cstreams = [(ypx, SEQ, 0, XBLK)] + ([] if last else [(ypc, CTX, SEQ, [(0, 256)])])
        dg = P.alloc("dg", [128, 2, 31, 128], BF16)
        for c in range(2):
            for jj in range(31):
                P.ts(dg[:, c, jj, :], identb(), vcol(l, V_CW + c * 31 + jj), ALU.mult)
        ycv = P.alloc("ycv", [128, 2, 512], F32)
        ybf = P.alloc("ybf", [128, 2, 512], BF16)
        sqy = P.alloc("sqy", [128, 2, 512], BF16)
        mu = P.alloc("mu", [128, 512], F32)
        var = P.alloc("var", [128, 512], F32)
        rsd = P.alloc("rsd", [128, 512], F32)
        dd_ = P.alloc("dd", [128, 512], F32)
        for (yp, T, goff, blks) in cstreams:
            for (b0, n) in blks:
                for c in range(2):
                    bk = 1 + c
                    for jj in range(31):
                        P.mm(ps[:, bk, 0:n], dg[:, c, jj, :], yp[:, c, b0 + jj:b0 + jj + n], jj == 0, jj == 30)
                    P.act(ycv[:, c, 0:n], ps[:, bk, 0:n], AF.Identity, bias=vcol(l, V_CB + c))
                    P.act(sqy[:, c, 0:n], ps[:, bk, 0:n], AF.Square, bias=vcol(l, V_CB + c))
                    P.copy(ybf[:, c, 0:n], ycv[:, c, 0:n], eng="pool")
                for c in range(2):
                    P.mm(ps[:, 3, 0:n], ones(), ybf[:, c, 0:n], c == 0, c == 1)
                for c in range(2):
                    P.mm(ps[:, 4, 0:n], ones(), sqy[:, c, 0:n], c == 0, c == 1)
                P.ts(mu[:, 0:n], ps[:, 3, 0:n], 1.0 / 256, ALU.mult)
                P.tt(var[:, 0:n], mu[:, 0:n], mu[:, 0:n], ALU.mult)
                P.stt(var[:, 0:n], ps[:, 4, 0:n], 1.0 / 256, var[:, 0:n], ALU.mult, ALU.subtract)
                rstd_from(rsd[:, 0:n], var[:, 0:n], 128, n, 1.0)
                for c in range(2):
                    P.tt(dd_[:, 0:n], ycv[:, c, 0:n], mu[:, 0:n], ALU.subtract)
                    P.tt(dd_[:, 0:n], dd_[:, 0:n], rsd[:, 0:n], ALU.mult)
                    P.act(mixcf[:, c, goff + b0:goff + b0 + n], dd_[:, 0:n], AF.Silu,
                          scale=vcol(l, V_LNG + c), bias=vcol(l, V_LNB + c))
        P.release(dg, ycv, ybf, sqy, mu, var, rsd, dd_, ypx, ypc)

        AB = P.alloc("AB", [128, 18, 512], BF16)
        ntc = 16 if last else 18
        for tc in range(ntc):
            bk = 1 + tc % 4
            for kc in range(2):
                P.mm(ps[:, bk, kc * 256:(kc + 1) * 256], uT[:, kc, tc * 128:(tc + 1) * 128], ccsc, True, True)
            P.copy(AB[:, tc, :], ps[:, bk, :], eng=("act" if tc % 2 else "dve"))
        P.release(uT)
        dft = [P.alloc("dft", [128, 2, 4, 1024], BF16) for _ in range(2)]
        sc_x = 1.0 / np.sqrt(SEQ * 64.0)

        def mk_dft(jn):
            half, sgp = jn // 4, jn % 4
            sl = dft[jn % 2]

            def ld():
                rows = slice(sgp * 512, (sgp + 1) * 512)
                cols = slice(half * 1024, (half + 1) * 1024)
                wload([sl[:, 0, :, :], sl[:, 1, :, :]],
                      [dftc_d[rows, cols].rearrange("(i p) c -> p i c", p=128),
                       dfts_d[rows, cols].rearrange("(i p) c -> p i c", p=128)], "dft%d" % (jn % 2), q="sp")

            def cp():
                for i in range(4):
                    scn = sgp * 4 + i
                    for c in range(2):
                        for sb in range(2):
                            bk = 1 + c * 2 + sb
                            P.mm(ps[:, bk, :], AB[:, scn, c * 256:c * 256 + 128], sl[:, 0, i, sb * 512:(sb + 1) * 512],
                                 scn == 0, False)
                            P.mm(ps[:, bk, :], AB[:, scn, c * 256 + 128:c * 256 + 256], sl[:, 1, i, sb * 512:(sb + 1) * 512],
                                 False, scn == 15)
                if sgp == 3:
                    for c in range(2):
                        for sb in range(2):
                            bk = 1 + c * 2 + sb
                            t0 = half * 1024 + sb * 512
                            P.act(mixcf[:, 2 + c, t0:t0 + 512], ps[:, bk, :], AF.Copy, scale=sc_x)
            return (ld, cp)

        run_stream([mk_dft(jn) for jn in range(8)])
        if not last:
            for c in range(2):
                bk = 5 + c
                for scn in range(2):
                    P.mm(ps[:, bk, 0:256], AB[:, 16 + scn, c * 256:c * 256 + 128], d256[:, scn, 0:256], scn == 0, False)
                    P.mm(ps[:, bk, 0:256], AB[:, 16 + scn, c * 256 + 128:c * 256 + 256], d256[:, scn, 256:512], False, scn == 1)
                P.act(mixcf[:, 2 + c, SEQ:SEQ + 256], ps[:, bk, 0:256], AF.Copy, scale=1.0 / np.sqrt(CTX * 64.0))
        P.release(AB, *dft)
        dump_sb("mixcf%d" % l, mixcf, [128, 4, TT])

        def out_proj(mixbuf, w_out):
            for (src, tb0, t0s, n, j) in streams:
                if last and j == 1:
                    continue
                g0 = tb0 + t0s
                for m in range(8):
                    bk = 1 + m % 6
                    for kk in range(4):
                        P.mm(ps[:, bk, 0:n], w_out[:, kk, m * 128:(m + 1) * 128], mixbuf[:, kk, g0:g0 + n], kk == 0, kk == 3)
                    P.stt(src[:, m, t0s:t0s + n], ps[:, bk, 0:n], mod[:, 16 + m, j:j + 1], src[:, m, t0s:t0s + n], ALU.mult, ALU.add)

        out_proj(mixcf, w_out)
        P.release(mixcf, w_out)

        mixa = P.alloc("mixa", [128, 4, TT], BF16)
        wq = P.alloc("wq", [128, 3, 768], BF16)
        wkn = P.alloc("wkn", [128, 2, 8, 96], BF16)
        wv = P.alloc("wv", [128, 2, 8, 64], BF16)
        P.memset(wkn[:, :, :, 64:96], 0.0, eng="pool")
        with nc.allow_non_contiguous_dma(reason="mla weight split"):
            ov, iv = [wq[:, :, :]], [w_uq_d[l].rearrange("(k p) c -> p k c", p=128)]
            for kc in range(2):
                src = w_ukv_d[l, kc * 128:(kc + 1) * 128, :].rearrange("p (h c) -> p h c", h=8)
                ov += [wkn[:, kc, :, 0:64], wv[:, kc, :, :]]
                iv += [src[:, :, 0:64], src[:, :, 64:128]]
            wload(ov, iv, "w_mla")
        qf = [P.alloc("qf", [96, TT], BF16) for _ in range(2)]
        kf = [P.alloc("kf", [96, TT], BF16) for _ in range(2)]
        va = [P.alloc("va", [128, 18, 128], BF16) for _ in range(2)]
        P.memset(va[0][:, :, 64:128], 1.0, eng="pool")
        P.memset(va[1][:, :, 0:64], 1.0, eng="pool")
        NSET = 3
        tset = [dict(sq1=P.alloc("sq1", [96, 512], BF16), qg=P.alloc("qg", [96, 512], BF16),
                     rs1=P.alloc("rs1", [96, 512], F32), t1=P.alloc("t1", [96, 512], F32),
                     t2=P.alloc("t2", [96, 512], F32)) for _ in range(NSET)]
        pt = [P.alloc("pt", [128, 512], BF16) for _ in range(4)]
        rec = [P.alloc("rec", [128, 512], F32) for _ in range(2)]
        Ct = lambda g0, n: rope[0:96, g0:g0 + n]
        St = lambda g0, n: rope[0:96, TT + g0:TT + g0 + n]
        scale_qk = 96.0 ** -0.5
        stepc = [0]

        def normrope_a(T, srcv, gcol, n):
            sq1, qg = T["sq1"], T["qg"]
            P.ts(qg[:, 0:n], srcv, gcol, ALU.mult)
            if srcv.sp == "PS":
                P.act(sq1[:, 0:n], srcv, AF.Square)
            else:
                P.tt(sq1[:, 0:n], srcv, srcv, ALU.mult, eng="pool")

        def normrope_b(T, g0, n, outv, pb):
            sq1, qg, rs1, t1, t2 = T["sq1"], T["qg"], T["rs1"], T["t1"], T["t2"]
            P.mm(ps[0:96, 7, 0:n], ones(96, 96), sq1[:, 0:n], True, True)
            P.mm(ps[0:96, pb, 0:n], permv, qg[:, 0:n], True, True)
            P.act(rs1[:, 0:n], ps[0:96, 7, 0:n], AF.Ln, scale=1.0 / 96, bias=epsc[0:96, 0:1])
            P.act(rs1[:, 0:n], rs1[:, 0:n], AF.Exp, scale=-0.5)
            P.tt(t1[:, 0:n], qg[:, 0:n], Ct(g0, n), ALU.mult)
            P.tt(t2[:, 0:n], ps[0:96, pb, 0:n], St(g0, n), ALU.mult)

        def normrope_c1(T, n):
            P.tt(T["t1"][:, 0:n], T["t1"][:, 0:n], T["t2"][:, 0:n], ALU.add, eng="pool")

        def normrope_c2(T, n, outv):
            P.tt(outv, T["t1"][:, 0:n], T["rs1"][:, 0:n], ALU.mult)

        allblk = [(b0, n) for (b0, n) in XBLK] + [CBLK]

        def prep_steps(h):
            hp = h % 2
            steps = []

            def kstep(g0, n):
                st = {}

                def fa():
                    T = tset[stepc[0] % NSET]; stepc[0] += 1
                    st["T"] = T
                    for kc in range(2):
                        P.mm(ps[0:96, 5, 0:n], wkn[:, kc, h, :], ckvn[:, kc, g0:g0 + n], kc == 0, kc == 1)
                    P.tt(T["t1"][:, 0:n], ps[0:96, 5, 0:n], krope[0:96, g0:g0 + n], ALU.add)
                    normrope_a(T, T["t1"][:, 0:n], vcol(l, V_KN, 1, 96), n)

                def fb():
                    normrope_b(st["T"], g0, n, kf[hp][:, g0:g0 + n], 6)
                return (fa, fb, lambda: normrope_c1(st["T"], n), lambda: normrope_c2(st["T"], n, kf[hp][:, g0:g0 + n]))

            def qstep(g0, n):
                st = {}

                def fa():
                    T = tset[stepc[0] % NSET]; stepc[0] += 1
                    st["T"] = T
                    for kc in range(3):
                        P.mm(ps[0:96, 5, 0:n], wq[:, kc, h * 96:(h + 1) * 96], cqn[:, kc, g0:g0 + n], kc == 0, kc == 2)
                    normrope_a(T, ps[0:96, 5, 0:n], vcol(l, V_QN, 1, 96), n)

                def fb():
                    normrope_b(st["T"], g0, n, qf[hp][:, g0:g0 + n], 6)
                return (fa, fb, lambda: normrope_c1(st["T"], n), lambda: normrope_c2(st["T"], n, qf[hp][:, g0:g0 + n]))

            def vstep(j0):
                def f():
                    vo = 0 if hp == 0 else 64
                    nj = min(8, 18 - j0)
                    for jj in range(nj):
                        j = j0 + jj
                        for kc in range(2):
                            P.mm(ps[:, 5, jj * 64:(jj + 1) * 64], ckvn[:, kc, j * 128:(j + 1) * 128], wv[:, kc, h, :], kc == 0, kc == 1)
                    P.copy(va[hp][:, j0:j0 + nj, vo:vo + 64],
                           ps[:, 5, 0:nj * 64].w(lambda a: a.rearrange("p (j c) -> p j c", c=64)), eng="dve")
                return (f, None, None, None)

            for (g0, n) in allblk:
                steps.append(kstep(g0, n))
            for (g0, n) in allblk:
                if last and g0 >= SEQ:
                    continue
                steps.append(qstep(g0, n))
            for j0 in (0, 8, 16):
                steps.append(vstep(j0))
            return steps

        class Prep:
            def __init__(s_, steps):
                s_.steps = list(steps)
                s_.p1 = None
                s_.p2 = None

            def tick(s_):
                c = s_.p2
                b_ = s_.p1
                nxt = s_.steps.pop(0) if s_.steps else None
                if c is not None and c[2] is not None:
                    c[2]()
                if nxt is not None:
                    nxt[0]()
                if b_ is not None and b_[1] is not None:
                    b_[1]()
                if c is not None and c[3] is not None:
                    c[3]()
                s_.p2 = b_
                s_.p1 = nxt

            def flush(s_):
                while s_.steps or s_.p1 is not None or s_.p2 is not None:
                    s_.tick()

        Prep(prep_steps(0)).flush()
        its = []
        for h in range(NH):
            qblocks = [(b0, n, list(range(18))) for (b0, n) in XBLK] + ([] if last else [(SEQ, 256, [16, 17])])
            for qi, (q0, nq, keys) in enumerate(qblocks):
                for ki, j in enumerate(keys):
                    its.append((h, qi, q0, nq, ki, j, len(keys)))
        LA = 2
        pend = {}
        for t in range(len(its) + LA):
            if t < len(its):
                h, qi, q0, nq, ki, j, nk = its[t]
                hp = h % 2
                if qi == 0 and ki == 0:
                    if h in pend:
                        pend.pop(h).flush()
                    if h + 1 < NH:
                        pend[h + 1] = Prep(prep_steps(h + 1))
                bs = 2 + (t % 3)
                P.mm(ps[:, bs, 0:nq], kf[hp][:, j * 128:(j + 1) * 128], qf[hp][:, q0:q0 + nq], True, True)
                P.act(pt[t % 4][:, 0:nq], ps[:, bs, 0:nq], AF.Exp, scale=scale_qk)
                if t % 4 == 3 and (h + 1) in pend:
                    pend[h + 1].tick()
            u = t - LA
            if u >= 0:
                h, qi, q0, nq, ki, j, nk = its[u]
                hp = h % 2
                bo = qi % 2
                P.mm(ps[:, bo, 0:nq], va[hp][:, j, :], pt[u % 4][:, 0:nq], ki == 0, ki == nk - 1)
                if ki == nk - 1:
                    no, do = (0, 64) if hp == 0 else (64, 0)
                    rc = rec[qi % 2]
                    P.recip(rc[no:no + 64, 0:nq], ps[do:do + 64, bo, 0:nq])
                    P.tt(mixa[no:no + 64, h // 2, q0:q0 + nq], ps[no:no + 64, bo, 0:nq], rc[no:no + 64, 0:nq], ALU.mult)
        tl = [v_ for T in tset for v_ in T.values()]
        P.release(wq, wkn, wv, *qf, *kf, *va, *tl, *pt, *rec, cqn, ckvn, krope)
        dump_sb("mixa%d" % l, mixa, [128, 4, TT])
        wr = None
        if last:
            wr = [P.alloc("wr", [128, 12288], BF16)]
            wload([wr[0][:, 0:4096].w(lambda a: a.rearrange("p (k f) -> p k f", k=8)),
                   wr[0][:, 4096:8192].w(lambda a: a.rearrange("p (k f) -> p k f", k=8)),
                   wr[0][:, 8192:12288].w(lambda a: a.rearrange("p (i d) -> p i d", i=4))],
                  [mg_d[0, 0][:, 0:512].rearrange("(k p) f -> p k f", p=128),
                   mu_d[0, 0][:, 0:512].rearrange("(k p) f -> p k f", p=128),
                   md_d[0, 0][0:512, :].rearrange("(i p) d -> p i d", p=128)], "wr0")
        if not last:
            wr = [P.alloc("wr", [128, 12288], BF16) for _ in range(2)]
            wload([wr[0][:, 0:4096].w(lambda a: a.rearrange("p (k f) -> p k f", k=8)),
                   wr[0][:, 4096:8192].w(lambda a: a.rearrange("p (k f) -> p k f", k=8)),
                   wr[0][:, 8192:12288].w(lambda a: a.rearrange("p (i d) -> p i d", i=4))],
                  [fg_d[0][:, 0:512].rearrange("(k p) f -> p k f", p=128),
                   fu_d[0][:, 0:512].rearrange("(k p) f -> p k f", p=128),
                   fd_d[0][0:512, :].rearrange("(i p) d -> p i d", p=128)], "wr0")
        w_outa = P.alloc("w_outa", [128, 4, D], BF16)
        wload([w_outa[:, :, :]], [w_out_d[l, 0:512, :].rearrange("(k p) c -> p k c", p=128)], "w_outa")
        out_proj(mixa, w_outa)
        P.release(mixa, w_outa)
        dump_sb("xTm%d" % l, xT, [128, 8, SEQ])

        hT = P.alloc("hT", [128, 8, TT], BF16)
        fstreams = [s_ for s_ in streams if not (last and s_[4] == 1)]
        if last:
            h32 = P.alloc("h32", [128, 8, 512], F32)
            rw = P.alloc("rw", [128, 8, NE], F32)
            lg = P.alloc("lg", [128, 16, NE], F32)
            with nc.allow_non_contiguous_dma(reason="router"):
                P.dma("sp", rw[:, :, :], rw_d[0].rearrange("(k p) e -> p k e", p=128), P.chan("rw"))
        for (src, tb0, t0s, n, j) in fstreams:
            g0 = tb0 + t0s
            adaln_block(src, t0s, n, gsf, 24, j, hT, h32=(h32 if last else None), ho=g0)
            if last:
                for tcn in range(n // 128):
                    for dc in range(8):
                        P.mm(ps[:, 6, tcn * 8:(tcn + 1) * 8], h32[:, dc, tcn * 128:(tcn + 1) * 128], rw[:, dc, :], dc == 0, dc == 7)
                tc0 = g0 // 128
                P.copy(lg[:, tc0:tc0 + 4, :], ps[:, 6, 0:32].w(lambda a: a.rearrange("p (t e) -> p t e", e=NE)))
        if last:
            P.release(h32, rw)
            m1 = P.alloc("m1", [128, 16], F32); m2 = P.alloc("m2", [128, 16], F32)
            eq1 = P.alloc("eq1", [128, 16, NE], F32); eq2 = P.alloc("eq2", [128, 16, NE], F32)
            l2 = P.alloc("l2", [128, 16, NE], F32); gts = P.alloc("gts", [128, 16, NE], F32)
            e2 = P.alloc("e2", [128, 16], F32); w1 = P.alloc("w1", [128, 16], F32); w2 = P.alloc("w2", [128, 16], F32)
            bc = lambda v: v.w(lambda a: a.unsqueeze(2).broadcast_to([128, 16, NE]))
            AX = mybir.AxisListType.X
            P.op("dve", lambda: nc.vector.tensor_reduce(out=m1[:, :].ap, in_=lg[:, :, :].ap, axis=AX, op=ALU.max),
                 reads=[lg[:, :, :]], writes=[m1[:, :]])
            P.tt(eq1[:, :, :], lg[:, :, :], bc(m1[:, :]), ALU.is_equal)
            P.stt(l2[:, :, :], eq1[:, :, :], -1e30, lg[:, :, :], ALU.mult, ALU.add)
            P.op("dve", lambda: nc.vector.tensor_reduce(out=m2[:, :].ap, in_=l2[:, :, :].ap, axis=AX, op=ALU.max),
                 reads=[l2[:, :, :]], writes=[m2[:, :]])
            P.tt(eq2[:, :, :], l2[:, :, :], bc(m2[:, :]), ALU.is_equal)
            P.tt(e2[:, :], m2[:, :], m1[:, :], ALU.subtract)
            P.act(e2[:, :], e2[:, :], AF.Exp)
            P.ts(w1[:, :], e2[:, :], 1.0, ALU.add)
            P.recip(w1[:, :], w1[:, :])
            P.tt(w2[:, :], e2[:, :], w1[:, :], ALU.mult)
            P.tt(gts[:, :, :], eq1[:, :, :], bc(w1[:, :]), ALU.mult)
            P.tt(eq2[:, :, :], eq2[:, :, :], bc(w2[:, :]), ALU.mult)
            P.tt(gts[:, :, :], gts[:, :, :], eq2[:, :, :], ALU.add)
            gT = P.alloc("gT", [8, SEQ], F32)
            selb = P.alloc("selb", [8, 1024], F32)
            P.dma("sp", selb[:, :], sel_d, P.chan("selb"))
            for tcn in range(16):
                bk = 1 + (tcn // 4) % 2
                P.transpose(ps[0:8, bk, (tcn % 4) * 128:(tcn % 4 + 1) * 128], gts[:, tcn, :], identf[:, :])
                if tcn % 4 == 3:
                    P.copy(gT[:, (tcn - 3) * 128:(tcn + 1) * 128], ps[0:8, bk, :])
            dump_sb("gT", gT, [8, SEQ])
            P.release(m1, m2, eq1, eq2, l2, e2, w1, w2, lg, gts)
            gb = P.alloc("gb", [128, SEQ], F32)

        pre0 = wr is not None
        if wr is None:
            wr = [P.alloc("wr", [128, 12288], BF16) for _ in range(2)]
        elif len(wr) == 1:
            wr.append(P.alloc("wr", [128, 12288], BF16))
        aT = [P.alloc("aT", [128, 4, 512], BF16) for _ in range(2)]
        sgt = [P.alloc("sgt", [128, 512], F32) for _ in range(2)]
        tmu = [P.alloc("tmu", [128, 512], F32) for _ in range(2)]
        groups = [(0, 4), (4, 4), (8, 4), (12, 4), (16, 4), (20, 2)]
        experts = [None] if not last else list(range(NE))
        jobs = []

        def mk_ffn(jobn, e, c0, ncg, first_of_expert):
            sl = wr[jobn % 2]
            f0 = c0 * 128
            nf = ncg * 128
            if e is None:
                Wg, Wu, Wd = fg_d[0], fu_d[0], fd_d[0]
            else:
                Wg, Wu, Wd = mg_d[0, e], mu_d[0, e], md_d[0, e]
            wgv = sl[:, 0:8 * nf].w(lambda a: a.rearrange("p (k f) -> p k f", k=8))
            wuv = sl[:, 4096:4096 + 8 * nf].w(lambda a: a.rearrange("p (k f) -> p k f", k=8))
            wdv = sl[:, 8192:8192 + ncg * 1024].w(lambda a: a.rearrange("p (i d) -> p i d", i=ncg))

            def ld():
                if jobn == 0 and pre0:
                    return
                wload([wgv, wuv, wdv],
                      [Wg[:, f0:f0 + nf].rearrange("(k p) f -> p k f", p=128),
                       Wu[:, f0:f0 + nf].rearrange("(k p) f -> p k f", p=128),
                       Wd[f0:f0 + nf, :].rearrange("(i p) d -> p i d", p=128)], "wr%d" % (jobn % 2))

            def cp():
                if e is not None and first_of_expert:
                    for bi, (b0, n) in enumerate(XBLK):
                        bk = 6 + bi % 2
                        P.mm(ps[:, bk, 0:n], selb[0:8, e * 128:(e + 1) * 128], gT[0:8, b0:b0 + n], True, True)
                        P.copy(gb[:, b0:b0 + n], ps[:, bk, 0:n], eng="act")
                wg3 = lambda dc, i: V(wgv.ap[:, dc, i * 128:(i + 1) * 128], wgv.sp, wgv.lo, wgv.hi)
                wu3 = lambda dc, i: V(wuv.ap[:, dc, i * 128:(i + 1) * 128], wuv.sp, wuv.lo, wuv.hi)
                wd3 = lambda i, m: V(wdv.ap[:, i, m * 128:(m + 1) * 128], wdv.sp, wdv.lo, wdv.hi)
                def gu_part(bi):
                    (src, tb0, t0s, n, j) = fstreams[bi]
                    g0 = tb0 + t0s
                    a_ = aT[bi % 2]
                    for i in range(ncg):
                        bg, bu = (0, 1) if i % 2 == 0 else (2, 3)
                        for dc in range(8):
                            P.mm(ps[:, bg, 0:n], wg3(dc, i), hT[:, dc, g0:g0 + n], dc == 0, dc == 7)
                        for dc in range(8):
                            P.mm(ps[:, bu, 0:n], wu3(dc, i), hT[:, dc, g0:g0 + n], dc == 0, dc == 7)
                        s_ = sgt[i % 2]
                        P.act(s_[:, 0:n], ps[:, bg, 0:n], AF.Silu)
                        if e is None:
                            P.tt(a_[:, i, 0:n], s_[:, 0:n], ps[:, bu, 0:n], ALU.mult)
                        else:
                            u_ = tmu[i % 2]
                            P.tt(u_[:, 0:n], ps[:, bu, 0:n], gb[:, g0:g0 + n], ALU.mult)
                            P.tt(a_[:, i, 0:n], s_[:, 0:n], u_[:, 0:n], ALU.mult, eng="pool")

                def down_part(bi):
                    (src, tb0, t0s, n, j) = fstreams[bi]
                    a_ = aT[bi % 2]
                    for m in range(8):
                        bk = 4 + m % 4
                        for i in range(ncg):
                            P.mm(ps[:, bk, 0:n], wd3(i, m), a_[:, i, 0:n], i == 0, i == ncg - 1)
                        P.stt(src[:, m, t0s:t0s + n], ps[:, bk, 0:n], mod[:, 40 + m, j:j + 1], src[:, m, t0s:t0s + n], ALU.mult, ALU.add)

                nb_ = len(fstreams)
                for bi in range(nb_ + 1):
                    if bi < nb_:
                        gu_part(bi)
                    if bi >= 1:
                        down_part(bi - 1)
            return (ld, cp)

        jn = 0
        for e in experts:
            for gi, (c0, ncg) in enumerate(groups):
                jobs.append(mk_ffn(jn, e, c0, ncg, gi == 0))
                jn += 1
        run_stream(jobs)
        P.release(hT, *wr, *aT, *sgt, *tmu)
        if last:
            P.release(gT, selb, gb)
        dump_sb("xTf%d" % l, xT, [128, 8, SEQ])

    osb = [P.alloc("osb", [128, D], F32) for _ in range(2)]
    co = [P.chan("o0"), P.chan("o1")]
    for i in range(16):
        ob = osb[i % 2]
        for half in range(2):
            bk = (2 * i + half) % 8
            for q in range(4):
                dc = half * 4 + q
                P.transpose(ps[:, bk, q * 128:(q + 1) * 128], xT[:, dc, i * 128:(i + 1) * 128], identf[:, :])
            P.copy(ob[:, half * 512:(half + 1) * 512], ps[:, bk, :], eng=("act" if half else "dve"))
        P.dma("sp", out_d[i * 128:(i + 1) * 128, :], ob[:, :], co[i % 2])
    fin = P.alloc("fin", [128, 16], F32)
    nw = P.emit()
    for ch in co + [c_ for n_, c_ in P.chans.items() if n_.startswith("out_")]:
        nc.sync.wait_ge(ch.sem, ch.count)
    return nc, dumps, len(P.ins), nw


_BUILT = {}


def make_inputs(inputs, b):
    c = consts()
    m = {"x": np.ascontiguousarray(inputs["x"][b], dtype=np.float32),
         "ctx": np.ascontiguousarray(inputs["ctx"][b], dtype=np.float32),
         "vecs": pack_vecs(inputs, b)}
    m.update(c)
    for k in ("ada_w", "w_in", "w_uq", "w_ukv", "w_out", "ffn_w_gate", "ffn_w_up", "ffn_w_down", "router_w",
              "moe_w_gate", "moe_w_up", "moe_w_down"):
        m[k] = np.ascontiguousarray(inputs[k], dtype=np.float32)
    return m


def kernel(**inputs):
    inputs = {k: np.asarray(v) for k, v in inputs.items()}
    if "nc" not in _BUILT:
        _BUILT["nc"] = build()
    nc = _BUILT["nc"][0]
    in_maps = [make_inputs(inputs, b) for b in range(8)]
    res = run_bass_kernel_spmd(nc, in_maps, core_ids=list(range(8)))
    return np.stack([np.asarray(r["out"], dtype=np.float32) for r in res.results], axis=0)
```
